# Optimizing a Trainium2 kernel written in Bass

```python
import jax
import jax.numpy as jnp
from jax import lax
import numpy as np

D_MODEL = 1024
BATCH = 4
SEQ = 8192
DEPTH = 1

CHUNK = 64
MIX_WIDTH = D_MODEL
GLA_HEADS = 4
GLA_DV = (MIX_WIDTH // 2) // GLA_HEADS
GLA_DK = GLA_DV // 2
GLA_GATE_RANK = 16
GLA_GATE_NORM = 16.0
RET_HEADS = 4
RET_DV = (MIX_WIDTH // 2) // RET_HEADS
RET_DK = RET_DV // 2
ROPE_BASE = 10000.0
N_EXPERTS = 32
TOP_K = 4
D_FF = D_MODEL
SWIGLU_ALPHA = 1.702
SWIGLU_LIMIT = 7.0
MOE_BLOCK = 256
LN_EPS = 1e-5
MAX_STREAM_OFFSET = 4096
DEEPNORM_ALPHA = (2.0 * DEPTH) ** 0.25
DEEPNORM_BETA = (8.0 * DEPTH) ** -0.25
PROJ_SIZES = (GLA_HEADS * GLA_DK, GLA_HEADS * GLA_DK, GLA_HEADS * GLA_DV, GLA_HEADS * GLA_DV, GLA_GATE_RANK,
              RET_HEADS * RET_DK, RET_HEADS * RET_DK, RET_HEADS * RET_DV, RET_HEADS * RET_DV)
PROJ_WIDTH = sum(PROJ_SIZES)
VALUE_BLOCKS = (2, 7)

kernel_name = 'hybrid_gla_retention_moe_deepnorm'


def layer_norm(x, g, b):
    xf = x.astype(jnp.float32)
    mu = jnp.mean(xf, -1, keepdims=True)
    var = jnp.mean(jnp.square(xf - mu), -1, keepdims=True)
    y = (xf - mu) * lax.rsqrt(var + LN_EPS) * g.astype(jnp.float32) + b.astype(jnp.float32)
    return y.astype(x.dtype)


def split_cols(t, sizes):
    idx = np.cumsum(sizes)[:-1].tolist()
    return jnp.split(t, idx, axis=-1)


def rotary(t, positions):
    half = t.shape[-1] // 2
    inv_freq = 1.0 / (ROPE_BASE ** jnp.linspace(0.0, 1.0, half, dtype=jnp.float32))
    ang = positions.astype(jnp.float32)[:, :, None, None] * inv_freq
    cos, sin = jnp.cos(ang), jnp.sin(ang)
    t1, t2 = t[..., :half], t[..., half:]
    return jnp.concatenate([t1 * cos - t2 * sin, t1 * sin + t2 * cos], axis=-1)


def chunk_decay_linear_attention(q, k, v, log_a):
    bsz, seq, heads, dk = q.shape
    dv = v.shape[-1]
    n = seq // CHUNK
    q = q.reshape(bsz, n, CHUNK, heads, dk)
    k = k.reshape(bsz, n, CHUNK, heads, dk)
    v = v.reshape(bsz, n, CHUNK, heads, dv)
    b = jnp.cumsum(log_a.reshape(bsz, n, CHUNK, heads, dk), axis=2)
    b_last = b[:, :, -1:]
    eb, enb = jnp.exp(b), jnp.exp(-b)
    qe = q * eb
    s_lo = jnp.einsum('bnihd,bnjhd->bnhij', qe, k * enb)
    s_up = jnp.einsum('bnihd,bnjhd->bnhij', q * enb, k * eb)
    lower = jnp.arange(CHUNK)[:, None] >= jnp.arange(CHUNK)[None, :]
    scores = jnp.where(lower, s_lo, s_up)
    o_intra = jnp.einsum('bnhij,bnjhe->bnihe', scores, v)
    kv = jnp.einsum('bnjhd,bnjhe->bnhde', k * jnp.exp(b_last - b), v)
    dec = jnp.exp(b_last[:, :, 0])

    def step(state, inp):
        kv_c, dec_c = inp
        return state * dec_c[..., None] + kv_c, state

    init = jnp.zeros((bsz, heads, dk, dv), jnp.float32)
    _, prev = lax.scan(step, init, (jnp.moveaxis(kv, 1, 0), jnp.moveaxis(dec, 1, 0)))
    prev = jnp.moveaxis(prev, 0, 1)
    o_inter = jnp.einsum('bnihd,bnhde->bnihe', qe, prev)
    return (o_intra + o_inter).reshape(bsz, seq, heads, dv)


def head_rms_norm(o, g):
    o = o * lax.rsqrt(jnp.mean(jnp.square(o), -1, keepdims=True) + LN_EPS)
    return (o * g).reshape(o.shape[0], o.shape[1], -1)


def head_group_norm(o, g, b):
    mu = jnp.mean(o, -1, keepdims=True)
    var = jnp.mean(jnp.square(o - mu), -1, keepdims=True)
    o = ((o - mu) * lax.rsqrt(var + LN_EPS)).reshape(o.shape[0], o.shape[1], -1)
    return o * g + b


def moe_ffn(h, router_w, router_b, w1, b1, w2, b2):
    bsz, seq, d = h.shape
    xt = h.reshape(-1, d)
    t = xt.shape[0]
    tk = t * TOP_K
    logits = (xt @ router_w + router_b).astype(jnp.float32)
    top_val, top_idx = lax.top_k(logits, TOP_K)
    gates = jax.nn.softmax(top_val, axis=-1)
    flat_e = top_idx.reshape(-1).astype(jnp.int32)
    flat_tok = jnp.repeat(jnp.arange(t, dtype=jnp.int32), TOP_K)
    flat_gate = gates.reshape(-1)
    order = jnp.argsort(flat_e)
    sorted_e = flat_e[order]
    counts = jnp.bincount(flat_e, length=N_EXPERTS).astype(jnp.int32)
    padded = ((counts + MOE_BLOCK - 1) // MOE_BLOCK) * MOE_BLOCK
    start_sorted = jnp.cumsum(counts) - counts
    pad_end = jnp.cumsum(padded)
    pad_start = pad_end - padded
    rank = jnp.arange(tk, dtype=jnp.int32) - start_sorted[sorted_e]
    dest = (pad_start[sorted_e] + rank).astype(jnp.int32)
    n_slots = ((tk + MOE_BLOCK - 1) // MOE_BLOCK) * MOE_BLOCK + N_EXPERTS * MOE_BLOCK
    n_blocks = n_slots // MOE_BLOCK
    slot_tok = jnp.zeros((n_slots,), jnp.int32).at[dest].set(flat_tok[order])
    slot_gate = jnp.zeros((n_slots,), jnp.float32).at[dest].set(flat_gate[order])
    block_start = jnp.arange(n_blocks, dtype=jnp.int32) * MOE_BLOCK
    block_expert = jnp.minimum(jnp.searchsorted(pad_end, block_start, side='right'),
                               N_EXPERTS - 1).astype(jnp.int32)

    def expert_block(args):
        tok, e = args
        xb = xt[tok]
        hh = xb @ w1[e] + b1[e]
        x_glu = jnp.minimum(hh[:, :D_FF], SWIGLU_LIMIT)
        x_lin = jnp.clip(hh[:, D_FF:], -SWIGLU_LIMIT, SWIGLU_LIMIT)
        act = x_glu * jax.nn.sigmoid(SWIGLU_ALPHA * x_glu) * (x_lin + 1.0)
        return act @ w2[e] + b2[e]

    y = lax.map(expert_block, (slot_tok.reshape(n_blocks, MOE_BLOCK), block_expert))
    y = y.reshape(n_slots, d) * slot_gate[:, None].astype(y.dtype)
    out = jnp.zeros((t, d), y.dtype).at[slot_tok].add(y)
    return out.reshape(bsz, seq, d)


def setup_inputs(seed: int = 0) -> dict:
    key = jax.random.key(seed)
    ks = jax.random.split(key, 24)
    f32 = jnp.float32

    def nrm(k, shape, scale):
        return jax.random.normal(k, shape, f32) * scale

    x = nrm(ks[0], (BATCH, SEQ, D_MODEL), 1.0)
    offsets = jax.random.randint(ks[1], (BATCH, 1), 0, MAX_STREAM_OFFSET, dtype=jnp.int32)
    positions = (offsets + jnp.arange(SEQ, dtype=jnp.int32)[None, :]).astype(jnp.int32)
    ln_in_g = 1.0 + nrm(ks[2], (D_MODEL,), 0.02)
    ln_in_b = nrm(ks[3], (D_MODEL,), 0.02)
    col_scale = jnp.concatenate([jnp.full((s,), DEEPNORM_BETA if i in VALUE_BLOCKS else 1.0, f32)
                                 for i, s in enumerate(PROJ_SIZES)])
    w_in = nrm(ks[4], (DEPTH, D_MODEL, PROJ_WIDTH), D_MODEL ** -0.5) * col_scale
    gla_gate_w = nrm(ks[5], (DEPTH, GLA_GATE_RANK, GLA_HEADS * GLA_DK), GLA_GATE_RANK ** -0.5)
    gla_gate_b = nrm(ks[6], (DEPTH, GLA_HEADS * GLA_DK), 0.1)
    gla_norm_g = 1.0 + nrm(ks[7], (DEPTH, GLA_DV), 0.02)
    ret_norm_g = 1.0 + nrm(ks[8], (DEPTH, RET_HEADS * RET_DV), 0.02)
    ret_norm_b = nrm(ks[9], (DEPTH, RET_HEADS * RET_DV), 0.02)
    w_out = nrm(ks[10], (DEPTH, MIX_WIDTH, D_MODEL), MIX_WIDTH ** -0.5 * DEEPNORM_BETA)
    ln1_g = 1.0 + nrm(ks[11], (DEPTH, D_MODEL), 0.02)
    ln1_b = nrm(ks[12], (DEPTH, D_MODEL), 0.02)
    router_w = nrm(ks[13], (DEPTH, D_MODEL, N_EXPERTS), D_MODEL ** -0.5)
    router_b = nrm(ks[14], (DEPTH, N_EXPERTS), 0.01)
    moe_w1 = nrm(ks[15], (DEPTH, N_EXPERTS, D_MODEL, 2 * D_FF), D_MODEL ** -0.5 * DEEPNORM_BETA)
    moe_b1 = nrm(ks[16], (DEPTH, N_EXPERTS, 2 * D_FF), 0.02)
    moe_w2 = nrm(ks[17], (DEPTH, N_EXPERTS, D_FF, D_MODEL), D_FF ** -0.5 * DEEPNORM_BETA)
    moe_b2 = nrm(ks[18], (DEPTH, N_EXPERTS, D_MODEL), 0.02)
    ln2_g = 1.0 + nrm(ks[19], (DEPTH, D_MODEL), 0.02)
    ln2_b = nrm(ks[20], (DEPTH, D_MODEL), 0.02)
    return {'x': x, 'positions': positions, 'ln_in_g': ln_in_g, 'ln_in_b': ln_in_b,
            'w_in': w_in, 'gla_gate_w': gla_gate_w, 'gla_gate_b': gla_gate_b,
            'gla_norm_g': gla_norm_g, 'ret_norm_g': ret_norm_g, 'ret_norm_b': ret_norm_b,
            'w_out': w_out, 'ln1_g': ln1_g, 'ln1_b': ln1_b, 'router_w': router_w,
            'router_b': router_b, 'moe_w1': moe_w1, 'moe_b1': moe_b1, 'moe_w2': moe_w2,
            'moe_b2': moe_b2, 'ln2_g': ln2_g, 'ln2_b': ln2_b}


def reference(x, positions, ln_in_g, ln_in_b, w_in, gla_gate_w, gla_gate_b, gla_norm_g,
              ret_norm_g, ret_norm_b, w_out, ln1_g, ln1_b, router_w, router_b,
              moe_w1, moe_b1, moe_w2, moe_b2, ln2_g, ln2_b):
    bsz, seq, _ = x.shape
    f32 = jnp.float32
    log_gamma = jnp.log1p(-jnp.exp2(-5.0 - jnp.arange(RET_HEADS, dtype=f32)))
    h = layer_norm(x, ln_in_g, ln_in_b)
    for l in range(DEPTH):
        proj = (h @ w_in[l]).astype(f32)
        gq, gk, gv, gg, glr, rq, rk, rv, rg = split_cols(proj, PROJ_SIZES)
        q = gq.reshape(bsz, seq, GLA_HEADS, GLA_DK) * GLA_DK ** -0.5
        k = gk.reshape(bsz, seq, GLA_HEADS, GLA_DK)
        v = gv.reshape(bsz, seq, GLA_HEADS, GLA_DV)
        log_a = jax.nn.log_sigmoid(glr @ gla_gate_w[l].astype(f32) + gla_gate_b[l].astype(f32)) / GLA_GATE_NORM
        log_a = log_a.reshape(bsz, seq, GLA_HEADS, GLA_DK)
        o_gla = chunk_decay_linear_attention(q, k, v, log_a)
        o_gla = head_rms_norm(o_gla, gla_norm_g[l].astype(f32)) * jax.nn.silu(gg)
        q = rotary(rq.reshape(bsz, seq, RET_HEADS, RET_DK), positions)
        k = rotary(rk.reshape(bsz, seq, RET_HEADS, RET_DK), positions) * RET_DK ** -0.5
        v = rv.reshape(bsz, seq, RET_HEADS, RET_DV)
        log_d = jnp.broadcast_to(log_gamma[None, None, :, None], q.shape)
        o_ret = chunk_decay_linear_attention(q, k, v, log_d)
        o_ret = head_group_norm(o_ret, ret_norm_g[l].astype(f32), ret_norm_b[l].astype(f32)) * jax.nn.silu(rg)
        mix = jnp.concatenate([o_gla, o_ret], axis=-1).astype(x.dtype) @ w_out[l]
        h = layer_norm(DEEPNORM_ALPHA * h + mix, ln1_g[l], ln1_b[l])
        ffn = moe_ffn(h, router_w[l], router_b[l], moe_w1[l], moe_b1[l], moe_w2[l], moe_b2[l])
        h = layer_norm(DEEPNORM_ALPHA * h + ffn.astype(h.dtype), ln2_g[l], ln2_b[l])
    return h
```

```python
import math
import os
from contextlib import ExitStack

import numpy as np
import concourse.bass as bass
import concourse.mybir as mybir
from concourse.bass_utils import run_bass_kernel_spmd

F32 = mybir.dt.float32
BF16 = mybir.dt.bfloat16
I32 = mybir.dt.int32
ALU = mybir.AluOpType
AF = mybir.ActivationFunctionType
AX = mybir.AxisListType

D = 1024
PW = 3088
NE = 32
ALPHA = 2.0 ** 0.25
EPS = 1e-5
TWO_PI = 2.0 * math.pi
C1 = 6.28125
C2 = TWO_PI - C1
LN8 = math.log(0.125)

CF_IDENT = 0
CF_TRI = 128
CF_SUF = 256
CF_LM = 384
CF_DT = 640
CF_STRICT = 1152
CF_ONES = 1280
CF_DQ = 1408
CF_DK = 1412
CF_DECR = 1416
CF_INVF = 1418
CF_EOFF = 1450
CF_N16 = 1482
CF_TOT = 1484


class Buf:
    __slots__ = ("wh", "wa", "r")

    def __init__(self):
        self.wh = {}
        self.wa = {}
        self.r = {}


def _mrg(d, k, s, v):
    if k not in d or d[k][1] < v:
        d[k] = (s, v)


class Sched:
    def __init__(self, nc, stack):
        self.nc = nc
        self.stack = stack
        self.eng = {"pe": nc.tensor, "dve": nc.vector, "act": nc.scalar, "pool": nc.gpsimd, "sp": nc.sync}
        self.sem = {}
        self.count = {}
        self.seen = {}
        for n in self.eng:
            self.sem[n] = stack.enter_context(nc.semaphore("s_" + n))
            self.count[n] = 0
            self.seen[n] = {}
        self.dsem = {}
        self.ninst = 0

    def _dsem(self, name):
        if name not in self.dsem:
            self.dsem[name] = [self.stack.enter_context(self.nc.semaphore("d_" + name)), 0]
        return self.dsem[name]

    def _waits(self, eng, need):
        e = self.eng[eng]
        for k, (s, v) in need.items():
            if k == eng and eng in ("pe", "sp"):
                continue
            if self.seen[eng].get(k, 0) < v:
                e.wait_ge(s, v)
                self.seen[eng][k] = v

    def emit(self, eng, fn, reads=(), writes=(), accw=(), dsem=None, sig=True):
        need = {}
        for b in reads:
            for dd in (b.wh, b.wa):
                for k, (s, v) in dd.items():
                    _mrg(need, k, s, v)
        for b in writes:
            for dd in (b.wh, b.wa, b.r):
                for k, (s, v) in dd.items():
                    _mrg(need, k, s, v)
        for b in accw:
            for dd in (b.wh, b.r):
                for k, (s, v) in dd.items():
                    _mrg(need, k, s, v)
        self._waits(eng, need)
        ins = fn(self.eng[eng])
        self.ninst += 1
        if dsem is None and not sig:
            tok = (eng, self.sem[eng], self.count[eng] + 1)
        elif dsem is None:
            self.count[eng] += 1
            ins.then_inc(self.sem[eng], 1)
            tok = (eng, self.sem[eng], self.count[eng])
        else:
            ds = self._dsem(dsem)
            ds[1] += 16
            ins.then_inc(ds[0], 16)
            tok = ("d_" + dsem, ds[0], ds[1])
        for b in reads:
            _mrg(b.r, *tok)
        for b in writes:
            b.wh = {tok[0]: (tok[1], tok[2])}
            b.wa = {}
            b.r = {}
        for b in accw:
            _mrg(b.wa, *tok)
        return tok

    def barrier(self):
        need = {}
        for n in self.eng:
            if self.count[n] > 0:
                need[n] = (self.sem[n], self.count[n])
        for name, (s, c) in self.dsem.items():
            if c > 0:
                need["d_" + name] = (s, c)
        for n in self.eng:
            nd = {k: v for k, v in need.items() if k != n}
            for k, (s, v) in nd.items():
                if self.seen[n].get(k, 0) < v:
                    self.eng[n].wait_ge(s, v)
                    self.seen[n][k] = v
            if n not in ("pe", "sp") and self.count[n] > 0:
                if self.seen[n].get(n, 0) < self.count[n]:
                    self.eng[n].wait_ge(self.sem[n], self.count[n])
                    self.seen[n][n] = self.count[n]


class _Stop(Exception):
    pass


class T:
    def __init__(self, t):
        self.t = t
        self.b = Buf()

    def __getitem__(self, k):
        return self.t[k]


def build_program(NT, NPRE, CAP):
    nc = bass.Bass("TRN2", target_bir_lowering=False)
    try:
        _build(nc, NT, NPRE, CAP)
    except _Stop:
        pass
    return nc


def _build(nc, NT, NPRE, CAP):
    NTT = NT + NPRE
    NSLOT = NE * CAP
    NBLK = CAP // 128
    nch = []
    if CAP <= 512:
        nch = [(0, CAP)]
    else:
        k = (CAP + 511) // 512
        step = ((CAP // k + 127) // 128) * 128 if (CAP // k) % 2 else CAP // k
        o = 0
        while o < CAP:
            nch.append((o, min(step, CAP - o)))
            o += step

    def din(name, shape, dt=F32):
        return nc.dram_tensor(name, shape, dt, kind="ExternalInput").ap()

    x_own = din("x_own", [NT * 128, D])
    x_pre = din("x_pre", [max(NPRE, 1) * 128, D])
    pos_in = din("pos", [128, NTT], I32)
    flag_in = din("flag", [128, 1])
    cf_in = din("cf", [128, CF_TOT])
    w_in_d = din("w_in", [D, PW])
    w_out_d = din("w_out", [D, D])
    gw_d = din("gate_w", [16, 256])
    gb_d = din("gate_b", [1, 256])
    rowp_d = din("rowp", [1, 6 * D + 128 + 512 + 512 + 32])
    rw_d = din("router_w", [D, NE])
    w1_d = din("moe_w1", [NE, D, 2 * D])
    b1_d = din("moe_b1T", [128, NE * 16])
    w2_d = din("moe_w2", [NE, D, D])
    b2_d = din("moe_b2", [NE, D])
    out_d = nc.dram_tensor("out", [NT * 128, D], F32, kind="ExternalOutput").ap()
    h1_d = nc.dram_tensor("h1_scr", [NT * 128, D], F32).ap()
    xs_d = nc.dram_tensor("xs_scr", [NSLOT, D], BF16).ap()
    ys_d = nc.dram_tensor("ys_scr", [NSLOT, D], F32).ap()
    h1_db, xs_db, ys_db, out_db = Buf(), Buf(), Buf(), Buf()

    with ExitStack() as top:
        S = Sched(nc, top)

        def sb(stack, name, shape, dt):
            return T(stack.enter_context(nc.sbuf_tensor("sb_" + name, shape, dt)))

        def ps(stack, name, shape, dt):
            return T(stack.enter_context(nc.psum_tensor(name, shape, dt)))

        def E(eng, fn, reads=(), writes=(), accw=(), dsem=None, sig=True):
            return S.emit(eng, fn, [x.b if isinstance(x, T) else x for x in reads],
                          [x.b if isinstance(x, T) else x for x in writes],
                          [x.b if isinstance(x, T) else x for x in accw], dsem, sig)

        def tt(eng, out, in0, in1, op, reads, writes):
            E(eng, lambda e: e.tensor_tensor(out=out, in0=in0, in1=in1, op=op), reads, writes)

        def ts(eng, out, in0, s1, s2, op0, op1, reads, writes):
            if op1 is None:
                E(eng, lambda e: e.tensor_scalar(out=out, in0=in0, scalar1=s1, scalar2=None, op0=op0), reads, writes)
            else:
                E(eng, lambda e: e.tensor_scalar(out=out, in0=in0, scalar1=s1, scalar2=s2, op0=op0, op1=op1),
                  reads, writes)

        def stt(out, in0, sc, in1, op0, op1, reads, writes):
            E("dve", lambda e: e.scalar_tensor_tensor(out=out, in0=in0, scalar=sc, in1=in1, op0=op0, op1=op1),
              reads, writes)

        def act(out, in_, func, reads, writes, bias=0.0, scale=1.0):
            E("act", lambda e: e.activation(out=out, in_=in_, func=func, bias=bias, scale=scale), reads, writes)

        def mm(out, lhsT, rhs, start, stop, reads, writes):
            E("pe", lambda e: e.matmul(out=out, lhsT=lhsT, rhs=rhs, start=start, stop=stop), reads, writes, sig=stop)

        def trp(out, in_, ident, reads, writes, sig=True):
            E("pe", lambda e: e.transpose(out=out, in_=in_, identity=ident), reads, writes, sig=sig)

        def dma(q, out, in_, reads, writes, sem, accw=()):
            E(q, lambda e: e.dma_start(out=out, in_=in_), reads, writes, accw, dsem=sem)

        cf = sb(top, "cf", [128, CF_TOT], F32)
        identb = sb(top, "identb", [128, 128], BF16)
        onesb = sb(top, "onesb", [128, 128], BF16)
        strictb = sb(top, "strictb", [128, 128], BF16)
        ln2g = sb(top, "ln2g", [128, D], F32)
        ln2b = sb(top, "ln2b", [128, D], F32)
        idx_tab = sb(top, "idx_tab", [128, NT, 4], I32)
        g_tab = sb(top, "g_tab", [128, NT, 4], F32)
        idx_b = [Buf() for _ in range(NT)]
        g_b = [Buf() for _ in range(NT)]
        pf = [ps(top, "pf%d" % i, [128, 512], F32) for i in range(6)]
        pb = [ps(top, "pb%d" % i, [128, 1024], BF16) for i in range(2)]
        pfi = [0]
        pbi = [0]

        def nf():
            pfi[0] += 1
            return pf[pfi[0] % NROT[0]]

        NROT = [6]

        def nb():
            pbi[0] += 1
            return pb[pbi[0] % 2]

        identf = cf[:, CF_IDENT:CF_IDENT + 128]
        bc_reg = nc.gpsimd.alloc_register("bc_reg")
        nc.gpsimd.reg_mov(bc_reg, NSLOT - 1)

        dma("sp", cf[:, :], cf_in[:, :], [], [cf], "c0")
        E("dve", lambda e: e.tensor_copy(out=identb[:, :], in_=cf[:, CF_IDENT:CF_IDENT + 128]), [cf], [identb])
        E("dve", lambda e: e.tensor_copy(out=onesb[:, :], in_=cf[:, CF_ONES:CF_ONES + 128]), [cf], [onesb])
        E("dve", lambda e: e.tensor_copy(out=strictb[:, :], in_=cf[:, CF_STRICT:CF_STRICT + 128]), [cf], [strictb])
        o_ = 4 * D
        dma("sp", ln2g[:, :], rowp_d[0:1, o_:o_ + D].broadcast_to([128, D]), [], [ln2g], "c1")
        dma("sp", ln2b[:, :], rowp_d[0:1, o_ + D:o_ + 2 * D].broadcast_to([128, D]), [], [ln2b], "c1")

        def layer_norm(stk_t, src, srcT, gT, bT, dst, dstT):
            st6, mv, sc2, xn = stk_t
            E("dve", lambda e: e.bn_stats(out=st6[:, 0, :], in_=src[:, 0:512]), [srcT], [st6])
            E("dve", lambda e: e.bn_stats(out=st6[:, 1, :], in_=src[:, 512:1024]), [srcT, st6], [st6])
            E("dve", lambda e: e.bn_aggr(out=mv[:, :], in_=st6[:, :, :].rearrange("p a b -> p (a b)")), [st6], [mv])
            act(sc2[:, 0:1], mv[:, 1:2], AF.Ln, [mv], [sc2], bias=epsb[:, 0:1])
            act(sc2[:, 0:1], sc2[:, 0:1], AF.Exp, [sc2], [sc2], scale=-0.5)
            ts("dve", sc2[:, 1:2], mv[:, 0:1], -1.0, sc2[:, 0:1], ALU.mult, ALU.mult, [mv, sc2], [sc2])
            act(xn[:, :], src, AF.Identity, [srcT, sc2], [xn], bias=sc2[:, 1:2], scale=sc2[:, 0:1])
            tt("dve", xn[:, :], xn[:, :], gT[:, :], ALU.mult, [xn, gT], [xn])
            tt("dve", dst, xn[:, :], bT[:, :], ALU.add, [xn, bT], [dstT])

        epsb = sb(top, "epsb", [128, 4], F32)
        E("dve", lambda e: e.memset(epsb[:, 0:1], EPS), [], [epsb])
        E("dve", lambda e: e.memset(epsb[:, 1:2], 1.0), [epsb], [epsb])
        E("dve", lambda e: e.memset(epsb[:, 2:3], LN8), [epsb], [epsb])

        KSTOP = int(os.environ.get("KSTOP", "9"))
        KSUB = int(os.environ.get("KSUB", "99"))
        KVAR = int(os.environ.get("KVAR", "0"))

        def stop_if(level):
            if KSTOP <= level:
                S.barrier()
                raise _Stop()

        with ExitStack() as pa:
            Win = sb(pa, "Win", [128, 8, PW], BF16)
            Wout = sb(pa, "Wout", [128, 8, D], BF16)
            gw = sb(pa, "gw", [16, 256], F32)
            gbr = sb(pa, "gbr", [1, 256], F32)
            rw = sb(pa, "rw", [128, 8, NE], F32)
            rbr = sb(pa, "rbr", [1, NE], F32)
            lnp = [sb(pa, "lnp%d" % i, [128, D], F32) for i in range(4)]
            gng = sb(pa, "gng", [128, 128], F32)
            rng = sb(pa, "rng", [128, 512], F32)
            rnb = sb(pa, "rnb", [128, 512], F32)
            flag = sb(pa, "flag", [128, 1], F32)
            posi = sb(pa, "posi", [128, NTT], I32)
            CCt = sb(pa, "CCt", [128, NTT, 32], F32)
            SNt = sb(pa, "SNt", [128, NTT, 32], F32)

            for c in range(8):
                for (a0, a1) in ((0, 1544), (1544, PW)):
                    dma("pool", Win[:, c, a0:a1], w_in_d[c * 128:(c + 1) * 128, a0:a1], [], [], "wl", accw=[Win])
            for c in range(8):
                dma("pool", Wout[:, c, :], w_out_d[c * 128:(c + 1) * 128, :], [], [], "wl", accw=[Wout])
            zt = sb(pa, "zt", [128, D], BF16)
            E("dve", lambda e: e.memset(zt[:, :], 0.0), [], [zt])
            KZ = NSLOT // 128
            xs_v = xs_d.rearrange("(p k) d -> p k d", p=128)
            zstep = min(8, KZ)
            for k0 in range(0, KZ, zstep):
                k1 = min(KZ, k0 + zstep)
                dma("pool", xs_v[:, k0:k1, :], zt[:, :].unsqueeze(1).broadcast_to([128, k1 - k0, D]), [zt], [], "zf",
                    accw=[xs_db])
            xs_db.wh, xs_db.wa = dict(xs_db.wa), {}

            dma("sp", gw[:, :], gw_d[:, :], [], [gw], "c1")
            dma("sp", gbr[:, :], gb_d[:, :], [], [gbr], "c1")
            dma("sp", rw[:, :, :], rw_d.rearrange("(c p) n -> p c n", p=128), [], [rw], "c1")
            o_ = 6 * D + 128 + 1024
            dma("sp", rbr[:, :], rowp_d[0:1, o_:o_ + NE], [], [rbr], "c1")
            for i in range(4):
                dma("sp", lnp[i][:, :], rowp_d[0:1, i * D:(i + 1) * D].broadcast_to([128, D]), [], [lnp[i]], "c1")
            o_ = 6 * D
            dma("sp", gng[:, :], rowp_d[0:1, o_:o_ + 128].broadcast_to([128, 128]), [], [gng], "c1")
            dma("sp", rng[:, :], rowp_d[0:1, o_ + 128:o_ + 640].broadcast_to([128, 512]), [], [rng], "c1")
            dma("sp", rnb[:, :], rowp_d[0:1, o_ + 640:o_ + 1152].broadcast_to([128, 512]), [], [rnb], "c1")
            dma("sp", flag[:, :], flag_in[:, :], [], [flag], "c1")
            dma("sp", posi[:, :], pos_in[:, :], [], [posi], "c1")

            with ExitStack() as pr:
                posf = sb(pr, "posf", [128, NTT], F32)
                ang = sb(pr, "ang", [128, NTT, 32], F32)
                a2 = sb(pr, "a2", [128, NTT, 32], F32)
                ki = sb(pr, "ki", [128, NTT, 32], I32)
                kf = sb(pr, "kf", [128, NTT, 32], F32)
                mk = sb(pr, "mk", [128, NTT, 32], F32)
                E("dve", lambda e: e.tensor_copy(out=posf[:, :], in_=posi[:, :]), [posi], [posf])
                tt("dve", ang[:, :, :], posf[:, :].unsqueeze(2).broadcast_to([128, NTT, 32]),
                   cf[:, CF_INVF:CF_INVF + 32].unsqueeze(1).broadcast_to([128, NTT, 32]), ALU.mult, [posf, cf], [ang])

                def sin_of(shift, outs):
                    ts("dve", a2[:, :, :], ang[:, :, :], shift, None, ALU.add, None, [ang], [a2])
                    ts("dve", ki[:, :, :], a2[:, :, :], 1.0 / TWO_PI, None, ALU.mult, None, [a2], [ki])
                    E("dve", lambda e: e.tensor_copy(out=kf[:, :, :], in_=ki[:, :, :]), [ki], [kf])
                    stt(a2[:, :, :], kf[:, :, :], -C1, a2[:, :, :], ALU.mult, ALU.add, [kf, a2], [a2])
                    stt(a2[:, :, :], kf[:, :, :], -C2, a2[:, :, :], ALU.mult, ALU.add, [kf, a2], [a2])
                    ts("dve", mk[:, :, :], a2[:, :, :], math.pi, -TWO_PI, ALU.is_gt, ALU.mult, [a2], [mk])
                    tt("dve", a2[:, :, :], a2[:, :, :], mk[:, :, :], ALU.add, [a2, mk], [a2])
                    ts("dve", mk[:, :, :], a2[:, :, :], -math.pi, TWO_PI, ALU.is_lt, ALU.mult, [a2], [mk])
                    tt("dve", a2[:, :, :], a2[:, :, :], mk[:, :, :], ALU.add, [a2, mk], [a2])
                    ts("dve", a2[:, :, :], a2[:, :, :], -math.pi, math.pi, ALU.max, ALU.min, [a2], [a2])
                    for (oap, neg, ob) in outs:
                        act(oap, a2[:, :, :], AF.Sin, [a2], [ob], scale=(-1.0 if neg else 1.0))

                sin_of(0.0, [(SNt[:, :, :], False, SNt)])
                sin_of(math.pi / 2, [(CCt[:, :, :], False, CCt)])
                S.barrier()

            def ring(name, shape, dt, n=1):
                if n == 1:
                    t_ = sb(pa, name, shape, dt)
                    return [t_, t_]
                return [sb(pa, "%s%d" % (name, i), shape, dt) for i in range(n)]

            xt = ring("xt", [128, D], F32, 2)
            lnt = [(sb(pa, "st6_0", [128, 2, 6], F32), sb(pa, "mv_0", [128, 2], F32),
                    sb(pa, "sc2_0", [128, 2], F32), sb(pa, "xn_0", [128, D], F32))] * 2
            h0 = ring("h0", [128, D], F32, 2)
            h0T = ring("h0T", [128, 8, 128], BF16, 2)
            glrT = ring("glrT", [16, 128], F32)
            el = ring("el", [128, 256], F32)
            ebx = ring("ebx", [128, 3, 256], F32)
            decg = ring("decg", [128, 2], F32)
            qkg = ring("qkg", [128, 4, 256], BF16)
            kdg = ring("kdg", [128, 256], BF16)
            rA = ring("rA", [128, 256], F32)
            rB = ring("rB", [128, 256], F32)
            rot = ring("rot", [128, 256], F32)
            qkr = ring("qkr", [128, 3, 256], BF16)
            kdr = ring("kdr", [128, 256], BF16)
            vg = ring("vg", [128, 512], BF16)
            vr = ring("vr", [128, 512], BF16)
            sgg = ring("sgg", [128, 512], F32, 2)
            sgr = ring("sgr", [128, 512], F32, 2)
            TT = ring("TT", [128, 14, 128], BF16)
            tmk = ring("tmk", [128, 2, 2, 128], F32)
            STg = ring("STg", [128, 4, 128], BF16)
            STr = ring("STr", [128, 4, 128], BF16)
            Sgf = sb(pa, "Sgf", [128, 2, 128], F32)
            Srf = sb(pa, "Srf", [128, 2, 128], F32)
            Sgb = [ring("Sgb_m%d" % m, [128, 2, 128], BF16, 2) for m in range(2)]
            Srb = [ring("Srb_m%d" % m, [128, 2, 128], BF16, 2) for m in range(2)]
            TM = [sb(pa, "TM%d" % m, [128, 6, 128], BF16) for m in range(2)]
            sq = ring("sq", [128, 512], F32)
            sm = ring("sm", [128, 8, 4], F32)
            on = ring("on", [128, 512], F32)
            cat = ring("cat", [128, D], BF16)
            catT = ring("catT", [128, 8, 128], BF16)
            rr = ring("rr", [128, D], F32)
            h1 = ring("h1", [128, D], F32)
            h1b = ring("h1b", [128, D], BF16)
            h1T = ring("h1T", [128, 8, 128], F32)
            rt = ring("rt", [128, 8, 32], F32)
            rs8 = ring("rs8", [128, 3, 8], F32)
            maskb = ring("maskb", [128, 32], BF16)
            base = sb(pa, "base", [128, 32], F32)

            NROT[0] = 4
            E("dve", lambda e: e.memset(Sgf[:, :, :], 0.0), [], [Sgf])
            E("dve", lambda e: e.memset(Srf[:, :, :], 0.0), [], [Srf])
            for m in range(2):
                E("dve", lambda e, m=m: e.memset(TM[m][:, :, :], 0.0), [], [TM[m]])
                for sl in range(2):
                    E("dve", lambda e, m=m, sl=sl: e.memset(Sgb[m][sl][:, :, :], 0.0), [], [Sgb[m][sl]])
                    E("dve", lambda e, m=m, sl=sl: e.memset(Srb[m][sl][:, :, :], 0.0), [], [Srb[m][sl]])
            E("dve", lambda e: e.memset(base[:, :], 0.0), [], [base])
            scur = [0]

            TRI = cf[:, CF_TRI:CF_TRI + 128]
            SUF = cf[:, CF_SUF:CF_SUF + 128]

            def rotary(src_ap, srcT, tg, i, dst_ap, dstT, dec_col, dec_ap, decT):
                sv = src_ap.rearrange("p (h d) -> p h d", d=64)
                A, B, R = rA[i], rB[i], rot[i]
                Av = A[:, :].rearrange("p (h d) -> p h d", d=64)
                Bv = B[:, :].rearrange("p (h d) -> p h d", d=64)
                Rv = R[:, :].rearrange("p (h d) -> p h d", d=64)
                tt("dve", A[:, :].rearrange("p (h t d) -> p h t d", h=4, t=2), src_ap.rearrange("p (h t d) -> p h t d", h=4, t=2),
                   CCt[:, tg, :].unsqueeze(1).unsqueeze(1).broadcast_to([128, 4, 2, 32]), ALU.mult, [srcT, CCt], [A])
                tt("dve", Bv[:, :, 0:32], sv[:, :, 32:64], SNt[:, tg, :].unsqueeze(1).broadcast_to([128, 4, 32]),
                   ALU.mult, [srcT, SNt], [B])
                tt("dve", Bv[:, :, 32:64], sv[:, :, 0:32], SNt[:, tg, :].unsqueeze(1).broadcast_to([128, 4, 32]),
                   ALU.mult, [srcT, SNt, B], [B])
                tt("dve", Rv[:, :, 0:32], Av[:, :, 0:32], Bv[:, :, 0:32], ALU.subtract, [A, B], [R])
                tt("dve", Rv[:, :, 32:64], Av[:, :, 32:64], Bv[:, :, 32:64], ALU.add, [A, B, R], [R])
                if dst_ap is not None:
                    act(dst_ap, R[:, :], AF.Copy, [R], [dstT])
                tt("dve", dec_ap.rearrange("p (h d) -> p h d", d=64), Rv,
                   cf[:, dec_col:dec_col + 4].unsqueeze(2).broadcast_to([128, 4, 64]), ALU.mult, [R, cf], [decT])

            def tile_pass(tg, main, tl):
                i = tg % 2
                xsrc = x_own if main else x_pre
                dma("sp", xt[i][:, :], xsrc[tl * 128:(tl + 1) * 128, :], [], [xt[i]], "x%d" % i)
                layer_norm(lnt[i], xt[i][:, :], xt[i], lnp[0], lnp[1], h0[i][:, :], h0[i])
                yield "F0"
                for half in range(2):
                    bk = nf()
                    for j in range(4):
                        c = half * 4 + j
                        trp(bk[:, j * 128:(j + 1) * 128], h0[i][:, c * 128:(c + 1) * 128], identf, [h0[i], cf], [bk], sig=(j == 3))
                    act(h0T[i][:, half * 4:half * 4 + 4, :], bk[:, :].rearrange("p (a b) -> p a b", b=128), AF.Copy,
                        [bk], [h0T[i]])

                def proj(g0, n):
                    bk = nf()
                    for c in range(8):
                        mm(bk[:, 0:n], h0T[i][:, c, :], Win[:, c, g0:g0 + n], c == 0, c == 7, [h0T[i], Win], [bk])
                    return bk

                bk = nf()
                for c in range(8):
                    mm(bk[0:16, 0:128], Win[:, c, 3072:3088], h0T[i][:, c, :], c == 0, c == 7, [h0T[i], Win], [bk])
                act(glrT[i][:, :], bk[0:16, 0:128], AF.Copy, [bk], [glrT[i]])
                bz = nf()
                mm(bz[:, 0:256], glrT[i][:, :], gw[:, :], True, False, [glrT[i], gw], [bz])
                mm(bz[:, 0:256], cf[0:1, CF_ONES:CF_ONES + 128], gbr[:, :], False, True, [cf, gbr], [bz])
                act(el[i][:, :], bz[:, 0:256], AF.Exp, [bz], [el[i]], scale=-1.0)
                act(el[i][:, :], el[i][:, :], AF.Ln, [el[i]], [el[i]], bias=epsb[:, 1:2])
                bc = nf()
                if main:
                    mm(bc[:, 0:256], TRI, el[i][:, :], True, True, [cf, el[i]], [bc])
                mm(bc[:, 256:512], SUF, el[i][:, :], True, True, [cf, el[i]], [bc])
                bd = nf()
                for hp in range(2):
                    mm(bd[:, 2 * hp:2 * hp + 2], el[i][:, hp * 128:(hp + 1) * 128], cf[:, CF_N16:CF_N16 + 2], True, True,
                       [el[i], cf], [bd])
                if main:
                    act(ebx[i][:, 0, :], bc[:, 0:256], AF.Exp, [bc], [ebx[i]])
                    act(ebx[i][:, 1, :], bc[:, 0:256], AF.Exp, [bc, ebx[i]], [ebx[i]], scale=-1.0)
                act(ebx[i][:, 2, :], bc[:, 256:512], AF.Exp, [bc, ebx[i]], [ebx[i]])
                for hp in range(2):
                    act(decg[i][:, hp:hp + 1], bd[:, 2 * hp:2 * hp + 1], AF.Exp, [bd, decg[i]], [decg[i]])

                if main:
                    yield "F1a"
                bV = proj(1024, 512)
                act(vg[i][:, :], bV[:, :], AF.Copy, [bV], [vg[i]])
                bV = proj(1536, 512)
                act(vr[i][:, :], bV[:, :], AF.Copy, [bV], [vr[i]])
                if main:
                    bG = proj(2048, 512)
                    act(sgg[i][:, :], bG[:, :], AF.Silu, [bG], [sgg[i]])
                    bG = proj(2560, 512)
                    act(sgr[i][:, :], bG[:, :], AF.Silu, [bG], [sgr[i]])
                bK = proj(0, 512)
                tt("dve", kdg[i][:, :], bK[:, 0:256], ebx[i][:, 2, :], ALU.mult, [bK, ebx[i]], [kdg[i]])
                if main:
                    tt("dve", qkg[i][:, 2, :], bK[:, 0:256], ebx[i][:, 0, :], ALU.mult, [bK, ebx[i]], [qkg[i]])
                    tt("dve", qkg[i][:, 3, :], bK[:, 0:256], ebx[i][:, 1, :], ALU.mult, [bK, ebx[i], qkg[i]], [qkg[i]])
                rotary(bK[:, 256:512], bK, tg, i, (qkr[i][:, 1, :] if main else None), qkr[i], CF_DK, kdr[i][:, :], kdr[i])
                if main:
                    bQ = proj(512, 512)
                    tt("dve", qkg[i][:, 0, :], bQ[:, 0:256], ebx[i][:, 0, :], ALU.mult, [bQ, ebx[i], qkg[i]], [qkg[i]])
                    tt("dve", qkg[i][:, 1, :], bQ[:, 0:256], ebx[i][:, 1, :], ALU.mult, [bQ, ebx[i], qkg[i]], [qkg[i]])
                    rotary(bQ[:, 256:512], bQ, tg, i, qkr[i][:, 0, :], qkr[i], CF_DQ, qkr[i][:, 2, :], qkr[i])
                if main:
                    yield "F1"
                sc = scur[0]
                sn = 1 - sc
                if main and KSUB > 1:
                    srcs = []
                    for v in range(4):
                        for hp in range(2):
                            srcs.append((qkg[i][:, v, hp * 128:(hp + 1) * 128], qkg[i]))
                    for v in (0, 1, 2):
                        for hp in range(2):
                            srcs.append((qkr[i][:, v, hp * 128:(hp + 1) * 128], qkr[i]))
                    for g0 in range(0, 14, 4):
                        n = min(4, 14 - g0)
                        bk = nb()
                        for j in range(n):
                            sap, sT = srcs[g0 + j]
                            trp(bk[:, j * 128:(j + 1) * 128], sap, identb[:, :], [sT, identb], [bk], sig=(j == n - 1))
                        E("dve", lambda e, bk=bk, g0=g0, n=n: e.tensor_copy(
                            out=TT[i][:, g0:g0 + n, :], in_=bk[:, 0:n * 128].rearrange("p (a b) -> p a b", b=128)),
                          [bk, TT[i]], [TT[i]])
                        if g0 == 0 or g0 == 8:
                            nm, o0 = (4, 0) if g0 == 0 else (2, 4)
                            for m in range(2):
                                E("dve", lambda e, m=m, bk=bk, o0=o0, nm=nm: e.tensor_copy(
                                    out=TM[m][64 * m:64 * m + 64, o0:o0 + nm, :],
                                    in_=bk[64 * m:64 * m + 64, 0:nm * 128].rearrange("p (a b) -> p a b", b=128)),
                                  [bk, TM[m]], [TM[m]])
                    for hp in range(2 if (KSUB > 2 and not (KVAR & 1)) else 0):
                        bk = nf()
                        bv = bk[:, :].rearrange("p (h v i) -> p h v i", h=2, v=2)
                        for hh in range(2):
                            p0 = 64 * hh
                            mm(bv[:, hh, 0, :], TT[i][:, 6 + hp, :], TM[hh][:, 0 + hp, :], True, True,
                               [TT[i], TM[hh]], [bk])
                            mm(bv[:, hh, 1, :], TT[i][:, 4 + hp, :], TM[hh][:, 2 + hp, :], True, True,
                               [TT[i], TM[hh]], [bk])
                        m2 = cf[:, CF_LM:CF_LM + 256].rearrange("p (v i) -> p v i", v=2)
                        for hh in range(2):
                            tt("dve", tmk[i][:, hh, :, :], bv[:, hh, :, :], m2, ALU.mult, [bk, cf, tmk[i]], [tmk[i]])
                        tt("dve", STg[i][:, 2 * hp:2 * hp + 2, :], tmk[i][:, :, 0, :], tmk[i][:, :, 1, :], ALU.add,
                           [tmk[i], STg[i]], [STg[i]])
                    bk = nf()
                    for h in range(4 if (KSUB > 2 and not (KVAR & 2)) else 0):
                        hp, p0 = h // 2, 64 * (h % 2)
                        mm(bk[:, h * 128:(h + 1) * 128], TT[i][:, 10 + hp, :], TM[h % 2][:, 4 + hp, :],
                           True, True, [TT[i], TM[h % 2]], [bk])
                    if not (KVAR & 2):
                      tt("dve", STr[i][:, :, :], bk[:, :].rearrange("p (h i) -> p h i", h=4),
                       cf[:, CF_DT:CF_DT + 512].rearrange("p (h i) -> p h i", h=4), ALU.mult, [bk, cf], [STr[i]])
                    bog = pf[4]
                    for h in range(4 if KSUB > 3 else 0):
                        hp, p0 = h // 2, 64 * (h % 2)
                        mm(bog[:, h * 128:(h + 1) * 128], STg[i][:, h, :], vg[i][:, h * 128:(h + 1) * 128], True, False,
                           [STg[i], vg[i]], [bog])
                        mm(bog[:, h * 128:(h + 1) * 128], TT[i][:, 0 + hp, :], Sgb[h % 2][sc][:, hp, :],
                           False, True, [TT[i], Sgb[h % 2][sc]], [bog])
                    bor = pf[5]
                    for h in range(4 if KSUB > 3 else 0):
                        hp, p0 = h // 2, 64 * (h % 2)
                        mm(bor[:, h * 128:(h + 1) * 128], STr[i][:, h, :], vr[i][:, h * 128:(h + 1) * 128], True, False,
                           [STr[i], vr[i]], [bor])
                        mm(bor[:, h * 128:(h + 1) * 128], TT[i][:, 12 + hp, :], Srb[h % 2][sc][:, hp, :],
                           False, True, [TT[i], Srb[h % 2][sc]], [bor])
                for (kd, vv, Sf, Sb, isg) in ((kdg[i], vg[i], Sgf, Sgb, True), (kdr[i], vr[i], Srf, Srb, False)):
                    bk = nf()
                    for hp in range(2):
                        mm(bk[:, hp * 256:(hp + 1) * 256], kd[:, hp * 128:(hp + 1) * 128], vv[:, hp * 256:(hp + 1) * 256],
                           True, True, [kd, vv], [bk])
                    for hp in range(2):
                        for hh in range(2):
                            p0 = 64 * hh
                            if isg:
                                dsc, dT = decg[i][p0:p0 + 64, hp:hp + 1], decg[i]
                            else:
                                dsc, dT = cf[p0:p0 + 64, CF_DECR + hp:CF_DECR + hp + 1], cf
                            stt(Sf[p0:p0 + 64, hp, :], Sf[p0:p0 + 64, hp, :], dsc,
                                bk[p0:p0 + 64, hp * 256 + hh * 128:hp * 256 + hh * 128 + 128], ALU.mult, ALU.add,
                                [Sf, dT, bk], [Sf])
                    if (not main) and tl == NPRE - 1:
                        ts("dve", Sf[:, :, :], Sf[:, :, :], flag[:, 0:1], None, ALU.mult, None, [Sf, flag], [Sf])
                    for m in range(2):
                        act(Sb[m][sn][64 * m:64 * m + 64, :, :], Sf[64 * m:64 * m + 64, :, :], AF.Copy, [Sf], [Sb[m][sn]])
                scur[0] = sn
                if not main or KSUB <= 4:
                    return
                yield "F2"
                smv = sm[i]
                act(sq[i][:, :], bog[:, :], AF.Square, [bog], [sq[i]])
                E("dve", lambda e: e.reduce_sum(out=smv[:, 0, :], in_=sq[i][:, :].rearrange("p (h d) -> p h d", h=4),
                                                axis=AX.X), [sq[i]], [smv])
                ts("dve", smv[:, 1, :], smv[:, 0, :], 1.0 / (128.0 * 64.0), EPS, ALU.mult, ALU.add, [smv], [smv])
                act(smv[:, 1, :], smv[:, 1, :], AF.Ln, [smv], [smv])
                act(smv[:, 1, :], smv[:, 1, :], AF.Exp, [smv], [smv], scale=-0.5, bias=epsb[:, 2:3])
                for h in range(4):
                    act(on[i][:, h * 128:(h + 1) * 128], bog[:, h * 128:(h + 1) * 128], AF.Identity, [bog, smv, on[i]],
                        [on[i]], scale=smv[:, 1, h:h + 1])
                tt("dve", on[i][:, :].rearrange("p (h d) -> p h d", h=4), on[i][:, :].rearrange("p (h d) -> p h d", h=4),
                   gng[:, :].unsqueeze(1).broadcast_to([128, 4, 128]), ALU.mult, [on[i], gng], [on[i]])
                tt("dve", cat[i][:, 0:512], on[i][:, :], sgg[i][:, :], ALU.mult, [on[i], sgg[i]], [cat[i]])
                E("dve", lambda e: e.reduce_sum(out=smv[:, 2, :], in_=bor[:, :].rearrange("p (h d) -> p h d", h=4),
                                                axis=AX.X), [bor, smv], [smv])
                act(sq[i][:, :], bor[:, :], AF.Square, [bor], [sq[i]])
                E("dve", lambda e: e.reduce_sum(out=smv[:, 3, :], in_=sq[i][:, :].rearrange("p (h d) -> p h d", h=4),
                                                axis=AX.X), [sq[i], smv], [smv])
                ts("dve", smv[:, 4, :], smv[:, 2, :], 1.0 / 128.0, None, ALU.mult, None, [smv], [smv])
                tt("dve", smv[:, 5, :], smv[:, 4, :], smv[:, 4, :], ALU.mult, [smv], [smv])
                stt(smv[:, 6, :], smv[:, 3, :], 1.0 / 128.0, smv[:, 5, :], ALU.mult, ALU.subtract, [smv], [smv])
                ts("dve", smv[:, 6, :], smv[:, 6, :], 1.0 / 64.0, EPS, ALU.mult, ALU.add, [smv], [smv])
                act(smv[:, 6, :], smv[:, 6, :], AF.Ln, [smv], [smv])
                act(smv[:, 6, :], smv[:, 6, :], AF.Exp, [smv], [smv], scale=-0.5, bias=epsb[:, 2:3])
                stt(smv[:, 7, :], smv[:, 4, :], -1.0, smv[:, 6, :], ALU.mult, ALU.mult, [smv], [smv])
                for h in range(4):
                    act(on[i][:, h * 128:(h + 1) * 128], bor[:, h * 128:(h + 1) * 128], AF.Identity, [bor, smv, on[i]],
                        [on[i]], scale=smv[:, 6, h:h + 1], bias=smv[:, 7, h:h + 1])
                tt("dve", on[i][:, :], on[i][:, :], rng[:, :], ALU.mult, [on[i], rng], [on[i]])
                tt("dve", on[i][:, :], on[i][:, :], rnb[:, :], ALU.add, [on[i], rnb], [on[i]])
                tt("dve", cat[i][:, 512:1024], on[i][:, :], sgr[i][:, :], ALU.mult, [on[i], sgr[i], cat[i]], [cat[i]])
                if KSUB <= 5:
                    return
                yield "B1a"
                for half in range(2):
                    bk = nb()
                    for j in range(4):
                        c = half * 4 + j
                        trp(bk[:, j * 128:(j + 1) * 128], cat[i][:, c * 128:(c + 1) * 128], identb[:, :],
                            [cat[i], identb], [bk], sig=(j == 3))
                    E("dve", lambda e, bk=bk, half=half: e.tensor_copy(
                        out=catT[i][:, half * 4:half * 4 + 4, :], in_=bk[:, 0:512].rearrange("p (a b) -> p a b", b=128)),
                      [bk, catT[i]], [catT[i]])
                for half in range(2):
                    bk = nf()
                    for c in range(8):
                        mm(bk[:, :], catT[i][:, c, :], Wout[:, c, half * 512:(half + 1) * 512], c == 0, c == 7,
                           [catT[i], Wout], [bk])
                    stt(rr[i][:, half * 512:(half + 1) * 512], h0[i][:, half * 512:(half + 1) * 512], ALPHA, bk[:, :],
                        ALU.mult, ALU.add, [h0[i], bk, rr[i]], [rr[i]])
                layer_norm(lnt[i], rr[i][:, :], rr[i], lnp[2], lnp[3], h1[i][:, :], h1[i])
                yield "B1"
                dma("sp", h1_d[tl * 128:(tl + 1) * 128, :], h1[i][:, :], [h1[i]], [], "h1o%d" % i, accw=[h1_db])
                act(h1b[i][:, :], h1[i][:, :], AF.Copy, [h1[i]], [h1b[i]])
                if KSUB <= 6:
                    return
                for half in range(2):
                    bk = nf()
                    for j in range(4):
                        c = half * 4 + j
                        trp(bk[:, j * 128:(j + 1) * 128], h1[i][:, c * 128:(c + 1) * 128], identf, [h1[i], cf], [bk], sig=(j == 3))
                    act(h1T[i][:, half * 4:half * 4 + 4, :], bk[:, :].rearrange("p (a b) -> p a b", b=128), AF.Copy,
                        [bk, h1T[i]], [h1T[i]])
                bk = nf()
                for c in range(8):
                    mm(bk[:, 0:NE], h1T[i][:, c, :], rw[:, c, :], c == 0, False, [h1T[i], rw], [bk])
                mm(bk[:, 0:NE], cf[0:1, CF_ONES:CF_ONES + 128], rbr[:, :], False, True, [cf, rbr], [bk])
                if KSUB <= 7:
                    return
                R_, r8 = rt[i], rs8[i]
                lg, msk, ex, exm, rk, vld, d1, dsm = [R_[:, j, :] for j in range(8)]
                E("dve", lambda e: e.tensor_copy(out=lg, in_=bk[:, 0:NE]), [bk], [R_])
                E("dve", lambda e: e.max(out=r8[:, 0, :], in_=lg), [R_], [r8])
                ts("dve", msk, lg, r8[:, 0, 3:4], None, ALU.is_ge, None, [R_, r8], [R_])
                ts("dve", r8[:, 2, 0:1], r8[:, 0, 0:1], -1.0, None, ALU.mult, None, [r8], [r8])
                act(ex, lg, AF.Exp, [R_, r8], [R_], bias=r8[:, 2, 0:1])
                tt("dve", exm, ex, msk, ALU.mult, [R_], [R_])
                E("dve", lambda e: e.reduce_sum(out=r8[:, 2, 1:2], in_=exm, axis=AX.X), [R_, r8], [r8])
                E("dve", lambda e: e.reciprocal(out=r8[:, 2, 2:3], in_=r8[:, 2, 1:2]), [r8], [r8])
                E("dve", lambda e: e.tensor_copy(out=maskb[i][:, :], in_=msk), [R_], [maskb[i]])
                bk2 = nf()
                mm(bk2[:, 0:32], strictb[:, :], maskb[i][:, :], True, True, [strictb, maskb[i]], [bk2])
                mm(bk2[:, 32:64], onesb[:, :], maskb[i][:, :], True, True, [onesb, maskb[i]], [bk2])
                tt("dve", rk, bk2[:, 0:32], base[:, :], ALU.add, [bk2, base, R_], [R_])
                tt("dve", base[:, :], bk2[:, 32:64], base[:, :], ALU.add, [bk2, base], [base])
                ts("dve", vld, rk, float(CAP), None, ALU.is_lt, None, [R_], [R_])
                tt("dve", vld, vld, msk, ALU.mult, [R_], [R_])
                tt("dve", d1, rk, cf[:, CF_EOFF:CF_EOFF + 32], ALU.add, [R_, cf], [R_])
                tt("dve", dsm, d1, vld, ALU.mult, [R_], [R_])
                stt(exm, exm, r8[:, 2, 2:3], vld, ALU.mult, ALU.mult, [R_, r8], [R_])
                E("dve", lambda e: e.max(out=r8[:, 1, :], in_=dsm), [R_, r8], [r8])
                ts("dve", idx_tab[:, tl, :], r8[:, 1, 0:4], -1.0, None, ALU.add, None, [r8], [idx_b[tl]])
                for j in range(4):
                    stt(d1, dsm, r8[:, 1, j:j + 1], exm, ALU.is_equal, ALU.mult, [R_, r8], [R_])
                    E("dve", lambda e, j=j: e.reduce_sum(out=g_tab[:, tl, j:j + 1], in_=d1, axis=AX.X),
                      [R_, g_b[tl]], [g_b[tl]])
                for j in range(4 if KSTOP > 2 else 0):
                    E("pool", lambda e, j=j: e.indirect_dma_start(
                        out=xs_d[:, :], out_offset=bass.IndirectOffsetOnAxis(ap=idx_tab[:, tl, j:j + 1], axis=0),
                        in_=h1b[i][:, :], in_offset=None, bounds_check=bc_reg, oob_is_err=False),
                      [idx_b[tl], h1b[i]], [], [xs_db], dsem="sct%d" % i)

            stop_if(0)
            pgens = [tile_pass(tp, False, tp) for tp in range(NPRE)]
            if NPRE > 0:
                next(pgens[0])
            for tp in range(NPRE):
                if tp + 1 < NPRE:
                    next(pgens[tp + 1])
                for _ in pgens[tp]:
                    pass
            stop_if(1)
            gens = [tile_pass(NPRE + tm, True, tm) for tm in range(NT)]

            def step(tm_):
                if 0 <= tm_ < NT:
                    return next(gens[tm_], None)
                return None

            for _ in range(4):
                step(0)
            step(1)
            for tm in range(NT):
                step(tm + 1)
                step(tm)
                step(tm + 1)
                step(tm)
                step(tm + 1)
                step(tm + 2)
                while step(tm) is not None:
                    pass
            NROT[0] = 6
            S.barrier()
            stop_if(3)

        with ExitStack() as pbk:
            W1b = [sb(pbk, "W1b%d" % i, [128, 8, 2 * D], BF16) for i in range(2)]
            W2b = [sb(pbk, "W2b%d" % i, [128, 8, D], BF16) for i in range(2)]
            b2b = [sb(pbk, "b2b%d" % i, [1, D], BF16) for i in range(2)]
            b1T = sb(pbk, "b1T", [128, NE * 16], F32)
            XT = [sb(pbk, "XT%d" % i, [128, 8, CAP], BF16) for i in range(2)]
            AT = [sb(pbk, "AT%d" % i, [128, 8, CAP], BF16) for i in range(2)]
            NW = max(n for _, n in nch)
            tg_ = [sb(pbk, "tg%d" % i, [128, NW], F32) for i in range(2)]
            tsg = [sb(pbk, "tsg%d" % i, [128, NW], F32) for i in range(2)]
            tl_ = [sb(pbk, "tl%d" % i, [128, NW], F32) for i in range(2)]
            Ysb = [sb(pbk, "Ysb%d" % i, [128, D], F32) for i in range(2)]
            dma("sp", b1T[:, :], b1_d[:, :], [], [b1T], "c1")
            b1P = sb(pbk, "b1P", [128, NE * 16], F32)
            ts("dve", b1P[:, :], b1T[:, :], 1.0, None, ALU.add, None, [b1T], [b1P])

            def load_w(e):
                s = e % 2
                for c in range(8):
                    if c == 0:
                        dma("pool", W1b[s][:, c, :], w1_d[e, c * 128:(c + 1) * 128, :], [], [W1b[s]], "w1_%d" % s)
                    else:
                        dma("pool", W1b[s][:, c, :], w1_d[e, c * 128:(c + 1) * 128, :], [], [], "w1_%d" % s,
                            accw=[W1b[s]])
                for c in range(8):
                    if c == 0:
                        dma("pool", W2b[s][:, c, :], w2_d[e, c * 128:(c + 1) * 128, :], [], [W2b[s]], "w2_%d" % s)
                    else:
                        dma("pool", W2b[s][:, c, :], w2_d[e, c * 128:(c + 1) * 128, :], [], [], "w2_%d" % s,
                            accw=[W2b[s]])
                dma("pool", b2b[s][:, :], b2_d[e:e + 1, :], [], [b2b[s]], "b2_%d" % s)

            XR = [sb(pbk, "XR%d" % i, [128, NBLK, D], BF16) for i in range(2)]

            def load_x(e):
                s = e % 2
                for blk in range(NBLK):
                    r0 = e * CAP + blk * 128
                    if blk == 0:
                        dma("sp", XR[s][:, blk, :], xs_d[r0:r0 + 128, :], [xs_db], [XR[s]], "xr%d" % s)
                    else:
                        dma("sp", XR[s][:, blk, :], xs_d[r0:r0 + 128, :], [xs_db], [], "xr%d" % s, accw=[XR[s]])

            def transposes(e):
                s = e % 2
                for blk in range(NBLK):
                    for half in range(2):
                        bk = nb()
                        for j in range(4):
                            c = half * 4 + j
                            trp(bk[:, j * 128:(j + 1) * 128], XR[s][:, blk, c * 128:(c + 1) * 128], identb[:, :],
                                [XR[s], identb], [bk], sig=(j == 3))
                        E("dve", lambda e_, bk=bk, half=half, blk=blk, s=s: e_.tensor_copy(
                            out=XT[s][:, half * 4:half * 4 + 4, blk * 128:(blk + 1) * 128],
                            in_=bk[:, 0:512].rearrange("p (a b) -> p a b", b=128)), [bk, XT[s]], [XT[s]])

            load_w(0)
            load_x(0)
            transposes(0)
            cnt = [0]
            for e in range(NE):
                s = e % 2
                if e + 1 < NE:
                    load_w(e + 1)
                    load_x(e + 1)
                ti = 0
                for k in range(8):
                    for (n0, nsz) in nch:
                        u = ti % 2
                        ti += 1
                        bg = nf()
                        for c in range(8):
                            mm(bg[:, 0:nsz], W1b[s][:, c, k * 128:(k + 1) * 128], XT[s][:, c, n0:n0 + nsz], c == 0, c == 7,
                               [W1b[s], XT[s]], [bg])
                        bl = nf()
                        for c in range(8):
                            mm(bl[:, 0:nsz], W1b[s][:, c, D + k * 128:D + (k + 1) * 128], XT[s][:, c, n0:n0 + nsz], c == 0,
                               c == 7, [W1b[s], XT[s]], [bl])
                        ts("dve", tg_[u][:, 0:nsz], bg[:, 0:nsz], b1T[:, e * 16 + k:e * 16 + k + 1], 7.0, ALU.add, ALU.min,
                           [bg, b1T], [tg_[u]])
                        act(tsg[u][:, 0:nsz], tg_[u][:, 0:nsz], AF.Sigmoid, [tg_[u]], [tsg[u]], scale=1.702)
                        ts("dve", tl_[u][:, 0:nsz], bl[:, 0:nsz], b1P[:, e * 16 + 8 + k:e * 16 + 8 + k + 1], 8.0, ALU.add,
                           ALU.min, [bl, b1P], [tl_[u]])
                        tt("dve", tg_[u][:, 0:nsz], tg_[u][:, 0:nsz], tsg[u][:, 0:nsz], ALU.mult, [tg_[u], tsg[u]], [tg_[u]])
                        stt(AT[s][:, k, n0:n0 + nsz], tl_[u][:, 0:nsz], -6.0, tg_[u][:, 0:nsz], ALU.max, ALU.mult,
                            [tg_[u], tl_[u], AT[s]], [AT[s]])
                if e + 1 < NE:
                    transposes(e + 1)
                for blk in range(NBLK):
                    yi = cnt[0] % 2
                    cnt[0] += 1
                    r0 = e * CAP + blk * 128
                    for half in range(2):
                        bk = nf()
                        for k in range(8):
                            mm(bk[:, :], AT[s][:, k, blk * 128:(blk + 1) * 128], W2b[s][:, k, half * 512:(half + 1) * 512],
                               k == 0, False, [AT[s], W2b[s]], [bk])
                        mm(bk[:, :], onesb[0:1, :], b2b[s][0:1, half * 512:(half + 1) * 512], False, True,
                           [onesb, b2b[s]], [bk])
                        act(Ysb[yi][:, half * 512:(half + 1) * 512], bk[:, :], AF.Copy, [bk, Ysb[yi]], [Ysb[yi]])
                    dma("sp", ys_d[r0:r0 + 128, :], Ysb[yi][:, :], [Ysb[yi]], [], "yo%d" % yi, accw=[ys_db])
            S.barrier()
        stop_if(4)

        with ExitStack() as pc:
            h1t = [sb(pc, "h1t%d" % i, [128, D], F32) for i in range(2)]
            yj = [[sb(pc, "yj%d_%d" % (i, j), [128, D], F32) for j in range(4)] for i in range(2)]
            acc = [sb(pc, "acc%d" % i, [128, D], F32) for i in range(2)]
            ot = [sb(pc, "ot%d" % i, [128, D], F32) for i in range(2)]
            lnt2 = [(sb(pc, "c_st6_0", [128, 2, 6], F32), sb(pc, "c_mv_0", [128, 2], F32),
                     sb(pc, "c_sc2_0", [128, 2], F32), sb(pc, "c_xn_0", [128, D], F32))] * 2
            for i in range(2):
                for j in range(4):
                    E("dve", lambda e, i=i, j=j: e.memset(yj[i][j][:, :], 0.0), [], [yj[i][j]])
            for t in range(NT):
                i = t % 2
                dma("sp", h1t[i][:, :], h1_d[t * 128:(t + 1) * 128, :], [h1_db], [h1t[i]], "h1i%d" % i)
                for j in range(4):
                    E("pool", lambda e, i=i, j=j, t=t: e.indirect_dma_start(
                        out=yj[i][j][:, :], out_offset=None, in_=ys_d[:, :],
                        in_offset=bass.IndirectOffsetOnAxis(ap=idx_tab[:, t, j:j + 1], axis=0),
                        bounds_check=bc_reg, oob_is_err=False),
                      [idx_b[t], ys_db], [yj[i][j]], dsem="g%d_%d" % (i, j))
                act(acc[i][:, :], h1t[i][:, :], AF.Copy, [h1t[i]], [acc[i]], scale=ALPHA)
                for j in range(4):
                    stt(acc[i][:, :], yj[i][j][:, :], g_tab[:, t, j:j + 1], acc[i][:, :], ALU.mult, ALU.add,
                        [yj[i][j], g_b[t], acc[i]], [acc[i]])
                layer_norm(lnt2[i], acc[i][:, :], acc[i], ln2g, ln2b, ot[i][:, :], ot[i])
                dma("sp", out_d[t * 128:(t + 1) * 128, :], ot[i][:, :], [ot[i]], [], "oo%d" % i, accw=[out_db])
            S.barrier()
        if os.environ.get("KDEBUG"):
            print("counts", S.count, {k: v[1] for k, v in S.dsem.items()}, "ninst", S.ninst)


def make_consts(CAP):
    cf = np.zeros((128, CF_TOT), np.float64)
    j = np.arange(128)[:, None]
    i = np.arange(128)[None, :]
    cf[:, CF_IDENT:CF_IDENT + 128] = (j == i)
    cf[:, CF_TRI:CF_TRI + 128] = (j <= i) * (-1.0 / 16.0)
    cf[:, CF_SUF:CF_SUF + 128] = (j > i) * (-1.0 / 16.0)
    cf[:, CF_LM:CF_LM + 128] = (j <= i)
    cf[:, CF_LM + 128:CF_LM + 256] = (j > i) & ((j // 64) == (i // 64))
    lg = np.log1p(-np.exp2(-5.0 - np.arange(4, dtype=np.float32)).astype(np.float32)).astype(np.float32).astype(np.float64)
    for h in range(4):
        same = (j // 64) == (i // 64)
        dt = np.where(same, np.exp(lg[h] * np.abs(i - j)), np.where(j < i, np.exp(lg[h] * (i - j)), 0.0))
        cf[:, CF_DT + h * 128:CF_DT + (h + 1) * 128] = dt
        cf[:, CF_DQ + h] = np.exp(lg[h] * (np.arange(128) + 1))
        cf[:, CF_DK + h] = np.exp(lg[h] * (127 - np.arange(128)))
    for hp in range(2):
        cf[0:64, CF_DECR + hp] = np.exp(lg[2 * hp] * 128)
        cf[64:128, CF_DECR + hp] = np.exp(lg[2 * hp + 1] * 128)
    cf[:, CF_STRICT:CF_STRICT + 128] = (j < i)
    cf[:, CF_ONES:CF_ONES + 128] = 1.0
    invf = (1.0 / (np.float32(10000.0) ** np.linspace(0.0, 1.0, 32, dtype=np.float32))).astype(np.float32)
    cf[:, CF_INVF:CF_INVF + 32] = invf[None, :]
    cf[:, CF_EOFF:CF_EOFF + 32] = (np.arange(32) * CAP + 1)[None, :]
    cf[:, CF_N16:CF_N16 + 2] = -1.0 / 16.0
    return cf.astype(np.float32)


def _col_perm():
    sizes = (256, 256, 512, 512, 16, 256, 256, 512, 512)
    offs = np.cumsum((0,) + sizes)
    blk = lambda b: np.arange(offs[b], offs[b + 1])
    return np.concatenate([blk(1), blk(6), blk(0), blk(5), blk(2), blk(7), blk(3), blk(8), blk(4)])


def prepare_inputs(inputs, n_cores, NT, NPRE, CAP, seq):
    f = lambda a: np.ascontiguousarray(np.asarray(a, dtype=np.float32))
    x = f(inputs["x"])
    pos = np.asarray(inputs["positions"]).astype(np.int32)
    perm = _col_perm()
    w_in = np.ascontiguousarray(f(inputs["w_in"])[0][:, perm])
    rowp = np.concatenate([f(inputs["ln_in_g"]).reshape(-1), f(inputs["ln_in_b"]).reshape(-1),
                           f(inputs["ln1_g"]).reshape(-1), f(inputs["ln1_b"]).reshape(-1),
                           f(inputs["ln2_g"]).reshape(-1), f(inputs["ln2_b"]).reshape(-1),
                           f(inputs["gla_norm_g"]).reshape(-1), f(inputs["ret_norm_g"]).reshape(-1),
                           f(inputs["ret_norm_b"]).reshape(-1), f(inputs["router_b"]).reshape(-1)])[None, :]
    b1 = f(inputs["moe_b1"])[0]
    b1T = np.ascontiguousarray(b1.reshape(NE, 16, 128).transpose(2, 0, 1).reshape(128, NE * 16))
    shared = {
        "cf": make_consts(CAP), "w_in": w_in, "w_out": f(inputs["w_out"])[0], "gate_w": f(inputs["gla_gate_w"])[0],
        "gate_b": f(inputs["gla_gate_b"]), "rowp": np.ascontiguousarray(rowp), "router_w": f(inputs["router_w"])[0],
        "moe_w1": f(inputs["moe_w1"])[0], "moe_b1T": b1T, "moe_w2": f(inputs["moe_w2"])[0], "moe_b2": f(inputs["moe_b2"])[0],
    }
    halves = seq // (NT * 128)
    in_maps = []
    for c in range(n_cores):
        b, hf = c // halves, c % halves
        s0 = hf * NT * 128
        m = dict(shared)
        m["x_own"] = np.ascontiguousarray(x[b, s0:s0 + NT * 128])
        if NPRE > 0:
            p0 = s0 - NPRE * 128 if hf > 0 else 0
            m["x_pre"] = np.ascontiguousarray(x[b, p0:p0 + NPRE * 128])
            ppre = pos[b, p0:p0 + NPRE * 128]
        else:
            m["x_pre"] = np.zeros((128, D), np.float32)
            ppre = np.zeros((0,), np.int32)
        pall = np.concatenate([ppre, pos[b, s0:s0 + NT * 128]])
        m["pos"] = np.ascontiguousarray(pall.reshape(-1, 128).T)
        m["flag"] = np.full((128, 1), 1.0 if hf > 0 else 0.0, np.float32)
        in_maps.append(m)
    return in_maps


def run(inputs, n_cores, NT, NPRE, CAP, seq, bsz):
    nc = build_program(NT, NPRE, CAP)
    in_maps = prepare_inputs(inputs, n_cores, NT, NPRE, CAP, seq)
    res = run_bass_kernel_spmd(nc, in_maps, core_ids=list(range(n_cores)))
    halves = seq // (NT * 128)
    out = np.zeros((bsz, seq, D), np.float32)
    for c in range(n_cores):
        b, hf = c // halves, c % halves
        out[b, hf * NT * 128:(hf + 1) * NT * 128] = res.results[c]["out"]
    return out


def kernel(**inputs):
    return run(inputs, 8, 32, 32, 640, 8192, 4)
```

```python
import math
import os
from contextlib import ExitStack

import numpy as np
import concourse.bass as bass
import concourse.mybir as mybir
from concourse.bass_utils import run_bass_kernel_spmd

F32 = mybir.dt.float32
BF16 = mybir.dt.bfloat16
I32 = mybir.dt.int32
ALU = mybir.AluOpType
AF = mybir.ActivationFunctionType
AX = mybir.AxisListType

D = 1024
PW = 3088
NE = 32
ALPHA = 2.0 ** 0.25
EPS = 1e-5
TWO_PI = 2.0 * math.pi
C1 = 6.28125
C2 = TWO_PI - C1
LN8 = math.log(0.125)

CF_IDENT = 0
CF_TRI = 128
CF_SUF = 256
CF_LM = 384
CF_DT = 640
CF_STRICT = 1152
CF_ONES = 1280
CF_DQ = 1408
CF_DK = 1412
CF_DECR = 1416
CF_INVF = 1418
CF_EOFF = 1450
CF_N16 = 1482
CF_TOT = 1484


class Buf:
    __slots__ = ("wh", "wa", "r")

    def __init__(self):
        self.wh = {}
        self.wa = {}
        self.r = {}


def _mrg(d, k, s, v):
    if k not in d or d[k][1] < v:
        d[k] = (s, v)


class Sched:
    def __init__(self, nc, stack):
        self.nc = nc
        self.stack = stack
        self.eng = {"pe": nc.tensor, "dve": nc.vector, "act": nc.scalar, "pool": nc.gpsimd, "sp": nc.sync}
        self.sem = {}
        self.count = {}
        self.seen = {}
        for n in self.eng:
            self.sem[n] = stack.enter_context(nc.semaphore("s_" + n))
            self.count[n] = 0
            self.seen[n] = {}
        self.dsem = {}
        self.ninst = 0

    def _dsem(self, name):
        if name not in self.dsem:
            self.dsem[name] = [self.stack.enter_context(self.nc.semaphore("d_" + name)), 0]
        return self.dsem[name]

    def _waits(self, eng, need):
        e = self.eng[eng]
        for k, (s, v) in need.items():
            if k == eng and eng in ("pe", "sp"):
                continue
            if self.seen[eng].get(k, 0) < v:
                e.wait_ge(s, v)
                self.seen[eng][k] = v

    def emit(self, eng, fn, reads=(), writes=(), accw=(), dsem=None, sig=True):
        need = {}
        for b in reads:
            for dd in (b.wh, b.wa):
                for k, (s, v) in dd.items():
                    _mrg(need, k, s, v)
        for b in writes:
            for dd in (b.wh, b.wa, b.r):
                for k, (s, v) in dd.items():
                    _mrg(need, k, s, v)
        for b in accw:
            for dd in (b.wh, b.r):
                for k, (s, v) in dd.items():
                    _mrg(need, k, s, v)
        self._waits(eng, need)
        ins = fn(self.eng[eng])
        self.ninst += 1
        if dsem is None and not sig:
            tok = (eng, self.sem[eng], self.count[eng] + 1)
        elif dsem is None:
            self.count[eng] += 1
            ins.then_inc(self.sem[eng], 1)
            tok = (eng, self.sem[eng], self.count[eng])
        else:
            ds = self._dsem(dsem)
            ds[1] += 16
            ins.then_inc(ds[0], 16)
            tok = ("d_" + dsem, ds[0], ds[1])
        for b in reads:
            _mrg(b.r, *tok)
        for b in writes:
            b.wh = {tok[0]: (tok[1], tok[2])}
            b.wa = {}
            b.r = {}
        for b in accw:
            _mrg(b.wa, *tok)
        return tok

    def barrier(self):
        need = {}
        for n in self.eng:
            if self.count[n] > 0:
                need[n] = (self.sem[n], self.count[n])
        for name, (s, c) in self.dsem.items():
            if c > 0:
                need["d_" + name] = (s, c)
        for n in self.eng:
            nd = {k: v for k, v in need.items() if k != n}
            for k, (s, v) in nd.items():
                if self.seen[n].get(k, 0) < v:
                    self.eng[n].wait_ge(s, v)
                    self.seen[n][k] = v
            if n not in ("pe", "sp") and self.count[n] > 0:
                if self.seen[n].get(n, 0) < self.count[n]:
                    self.eng[n].wait_ge(self.sem[n], self.count[n])
                    self.seen[n][n] = self.count[n]


class _Stop(Exception):
    pass


class T:
    def __init__(self, t):
        self.t = t
        self.b = Buf()

    def __getitem__(self, k):
        return self.t[k]


def build_program(NT, NPRE, CAP):
    nc = bass.Bass("TRN2", target_bir_lowering=False)
    try:
        _build(nc, NT, NPRE, CAP)
    except _Stop:
        pass
    return nc


def _build(nc, NT, NPRE, CAP):
    NTT = NT + NPRE
    NSLOT = NE * CAP
    NBLK = CAP // 128
    nch = []
    if CAP <= 512:
        nch = [(0, CAP)]
    else:
        k = (CAP + 511) // 512
        step = ((CAP // k + 127) // 128) * 128 if (CAP // k) % 2 else CAP // k
        o = 0
        while o < CAP:
            nch.append((o, min(step, CAP - o)))
            o += step

    def din(name, shape, dt=F32):
        return nc.dram_tensor(name, shape, dt, kind="ExternalInput").ap()

    x_own = din("x_own", [NT * 128, D])
    x_pre = din("x_pre", [max(NPRE, 1) * 128, D])
    pos_in = din("pos", [128, NTT], I32)
    flag_in = din("flag", [128, 1])
    cf_in = din("cf", [128, CF_TOT])
    w_in_d = din("w_in", [D, PW])
    w_out_d = din("w_out", [D, D])
    gw_d = din("gate_w", [16, 256])
    gb_d = din("gate_b", [1, 256])
    rowp_d = din("rowp", [1, 6 * D + 128 + 512 + 512 + 32])
    rw_d = din("router_w", [D, NE])
    w1_d = din("moe_w1", [NE, D, 2 * D])
    b1_d = din("moe_b1T", [128, NE * 16])
    w2_d = din("moe_w2", [NE, D, D])
    b2_d = din("moe_b2", [NE, D])
    out_d = nc.dram_tensor("out", [NT * 128, D], F32, kind="ExternalOutput").ap()
    h1_d = nc.dram_tensor("h1_scr", [NT * 128, D], F32).ap()
    xs_d = nc.dram_tensor("xs_scr", [NSLOT, D], BF16).ap()
    ys_d = nc.dram_tensor("ys_scr", [NSLOT, D], F32).ap()
    h1_db, xs_db, ys_db, out_db = Buf(), Buf(), Buf(), Buf()

    with ExitStack() as top:
        S = Sched(nc, top)

        def sb(stack, name, shape, dt):
            return T(stack.enter_context(nc.sbuf_tensor("sb_" + name, shape, dt)))

        def ps(stack, name, shape, dt):
            return T(stack.enter_context(nc.psum_tensor(name, shape, dt)))

        def E(eng, fn, reads=(), writes=(), accw=(), dsem=None, sig=True):
            return S.emit(eng, fn, [x.b if isinstance(x, T) else x for x in reads],
                          [x.b if isinstance(x, T) else x for x in writes],
                          [x.b if isinstance(x, T) else x for x in accw], dsem, sig)

        def tt(eng, out, in0, in1, op, reads, writes):
            E(eng, lambda e: e.tensor_tensor(out=out, in0=in0, in1=in1, op=op), reads, writes)

        def ts(eng, out, in0, s1, s2, op0, op1, reads, writes):
            if op1 is None:
                E(eng, lambda e: e.tensor_scalar(out=out, in0=in0, scalar1=s1, scalar2=None, op0=op0), reads, writes)
            else:
                E(eng, lambda e: e.tensor_scalar(out=out, in0=in0, scalar1=s1, scalar2=s2, op0=op0, op1=op1),
                  reads, writes)

        def stt(out, in0, sc, in1, op0, op1, reads, writes):
            E("dve", lambda e: e.scalar_tensor_tensor(out=out, in0=in0, scalar=sc, in1=in1, op0=op0, op1=op1),
              reads, writes)

        def act(out, in_, func, reads, writes, bias=0.0, scale=1.0):
            E("act", lambda e: e.activation(out=out, in_=in_, func=func, bias=bias, scale=scale), reads, writes)

        def mm(out, lhsT, rhs, start, stop, reads, writes):
            E("pe", lambda e: e.matmul(out=out, lhsT=lhsT, rhs=rhs, start=start, stop=stop), reads, writes, sig=stop)

        def trp(out, in_, ident, reads, writes, sig=True):
            E("pe", lambda e: e.transpose(out=out, in_=in_, identity=ident), reads, writes, sig=sig)

        def dma(q, out, in_, reads, writes, sem, accw=()):
            E(q, lambda e: e.dma_start(out=out, in_=in_), reads, writes, accw, dsem=sem)

        cf = sb(top, "cf", [128, CF_TOT], F32)
        identb = sb(top, "identb", [128, 128], BF16)
        onesb = sb(top, "onesb", [128, 128], BF16)
        strictb = sb(top, "strictb", [128, 128], BF16)
        ln2g = sb(top, "ln2g", [128, D], F32)
        ln2b = sb(top, "ln2b", [128, D], F32)
        idx_tab = sb(top, "idx_tab", [128, NT, 4], I32)
        g_tab = sb(top, "g_tab", [128, NT, 4], F32)
        idx_b = [Buf() for _ in range(NT)]
        g_b = [Buf() for _ in range(NT)]
        pf = [ps(top, "pf%d" % i, [128, 512], F32) for i in range(6)]
        pb = [ps(top, "pb%d" % i, [128, 1024], BF16) for i in range(2)]
        pfi = [0]
        pbi = [0]

        def nf():
            pfi[0] += 1
            return pf[pfi[0] % NROT[0]]

        NROT = [6]

        def nb():
            pbi[0] += 1
            return pb[pbi[0] % 2]

        identf = cf[:, CF_IDENT:CF_IDENT + 128]
        bc_reg = nc.gpsimd.alloc_register("bc_reg")
        nc.gpsimd.reg_mov(bc_reg, NSLOT - 1)

        dma("sp", cf[:, :], cf_in[:, :], [], [cf], "c0")
        E("dve", lambda e: e.tensor_copy(out=identb[:, :], in_=cf[:, CF_IDENT:CF_IDENT + 128]), [cf], [identb])
        E("dve", lambda e: e.tensor_copy(out=onesb[:, :], in_=cf[:, CF_ONES:CF_ONES + 128]), [cf], [onesb])
        E("dve", lambda e: e.tensor_copy(out=strictb[:, :], in_=cf[:, CF_STRICT:CF_STRICT + 128]), [cf], [strictb])
        o_ = 4 * D
        dma("sp", ln2g[:, :], rowp_d[0:1, o_:o_ + D].broadcast_to([128, D]), [], [ln2g], "c1")
        dma("sp", ln2b[:, :], rowp_d[0:1, o_ + D:o_ + 2 * D].broadcast_to([128, D]), [], [ln2b], "c1")

        def layer_norm(stk_t, src, srcT, gT, bT, dst, dstT):
            st6, mv, sc2, xn = stk_t
            E("dve", lambda e: e.bn_stats(out=st6[:, 0, :], in_=src[:, 0:512]), [srcT], [st6])
            E("dve", lambda e: e.bn_stats(out=st6[:, 1, :], in_=src[:, 512:1024]), [srcT, st6], [st6])
            E("dve", lambda e: e.bn_aggr(out=mv[:, :], in_=st6[:, :, :].rearrange("p a b -> p (a b)")), [st6], [mv])
            act(sc2[:, 0:1], mv[:, 1:2], AF.Ln, [mv], [sc2], bias=epsb[:, 0:1])
            act(sc2[:, 0:1], sc2[:, 0:1], AF.Exp, [sc2], [sc2], scale=-0.5)
            ts("dve", sc2[:, 1:2], mv[:, 0:1], -1.0, sc2[:, 0:1], ALU.mult, ALU.mult, [mv, sc2], [sc2])
            act(xn[:, :], src, AF.Identity, [srcT, sc2], [xn], bias=sc2[:, 1:2], scale=sc2[:, 0:1])
            tt("dve", xn[:, :], xn[:, :], gT[:, :], ALU.mult, [xn, gT], [xn])
            tt("dve", dst, xn[:, :], bT[:, :], ALU.add, [xn, bT], [dstT])

        epsb = sb(top, "epsb", [128, 4], F32)
        E("dve", lambda e: e.memset(epsb[:, 0:1], EPS), [], [epsb])
        E("dve", lambda e: e.memset(epsb[:, 1:2], 1.0), [epsb], [epsb])
        E("dve", lambda e: e.memset(epsb[:, 2:3], LN8), [epsb], [epsb])

        KSTOP = int(os.environ.get("KSTOP", "9"))
        KSUB = int(os.environ.get("KSUB", "99"))
        KVAR = int(os.environ.get("KVAR", "0"))

        def stop_if(level):
            if KSTOP <= level:
                S.barrier()
                raise _Stop()

        with ExitStack() as pa:
            Win = sb(pa, "Win", [128, 8, PW], BF16)
            Wout = sb(pa, "Wout", [128, 8, D], BF16)
            gw = sb(pa, "gw", [16, 256], F32)
            gbr = sb(pa, "gbr", [1, 256], F32)
            rw = sb(pa, "rw", [128, 8, NE], F32)
            rbr = sb(pa, "rbr", [1, NE], F32)
            lnp = [sb(pa, "lnp%d" % i, [128, D], F32) for i in range(4)]
            gng = sb(pa, "gng", [128, 128], F32)
            rng = sb(pa, "rng", [128, 512], F32)
            rnb = sb(pa, "rnb", [128, 512], F32)
            flag = sb(pa, "flag", [128, 1], F32)
            posi = sb(pa, "posi", [128, NTT], I32)
            CCt = sb(pa, "CCt", [128, NTT, 32], F32)
            SNt = sb(pa, "SNt", [128, NTT, 32], F32)

            for c in range(8):
                for (a0, a1) in ((0, 1544), (1544, PW)):
                    dma("pool", Win[:, c, a0:a1], w_in_d[c * 128:(c + 1) * 128, a0:a1], [], [], "wl", accw=[Win])
            for c in range(8):
                dma("pool", Wout[:, c, :], w_out_d[c * 128:(c + 1) * 128, :], [], [], "wl", accw=[Wout])
            zt = sb(pa, "zt", [128, D], BF16)
            E("dve", lambda e: e.memset(zt[:, :], 0.0), [], [zt])
            KZ = NSLOT // 128
            xs_v = xs_d.rearrange("(p k) d -> p k d", p=128)
            zstep = min(8, KZ)
            for k0 in range(0, KZ, zstep):
                k1 = min(KZ, k0 + zstep)
                dma("pool", xs_v[:, k0:k1, :], zt[:, :].unsqueeze(1).broadcast_to([128, k1 - k0, D]), [zt], [], "zf",
                    accw=[xs_db])
            xs_db.wh, xs_db.wa = dict(xs_db.wa), {}

            dma("sp", gw[:, :], gw_d[:, :], [], [gw], "c1")
            dma("sp", gbr[:, :], gb_d[:, :], [], [gbr], "c1")
            dma("sp", rw[:, :, :], rw_d.rearrange("(c p) n -> p c n", p=128), [], [rw], "c1")
            o_ = 6 * D + 128 + 1024
            dma("sp", rbr[:, :], rowp_d[0:1, o_:o_ + NE], [], [rbr], "c1")
            for i in range(4):
                dma("sp", lnp[i][:, :], rowp_d[0:1, i * D:(i + 1) * D].broadcast_to([128, D]), [], [lnp[i]], "c1")
            o_ = 6 * D
            dma("sp", gng[:, :], rowp_d[0:1, o_:o_ + 128].broadcast_to([128, 128]), [], [gng], "c1")
            dma("sp", rng[:, :], rowp_d[0:1, o_ + 128:o_ + 640].broadcast_to([128, 512]), [], [rng], "c1")
            dma("sp", rnb[:, :], rowp_d[0:1, o_ + 640:o_ + 1152].broadcast_to([128, 512]), [], [rnb], "c1")
            dma("sp", flag[:, :], flag_in[:, :], [], [flag], "c1")
            dma("sp", posi[:, :], pos_in[:, :], [], [posi], "c1")

            with ExitStack() as pr:
                posf = sb(pr, "posf", [128, NTT], F32)
                ang = sb(pr, "ang", [128, NTT, 32], F32)
                a2 = sb(pr, "a2", [128, NTT, 32], F32)
                ki = sb(pr, "ki", [128, NTT, 32], I32)
                kf = sb(pr, "kf", [128, NTT, 32], F32)
                mk = sb(pr, "mk", [128, NTT, 32], F32)
                E("dve", lambda e: e.tensor_copy(out=posf[:, :], in_=posi[:, :]), [posi], [posf])
                tt("dve", ang[:, :, :], posf[:, :].unsqueeze(2).broadcast_to([128, NTT, 32]),
                   cf[:, CF_INVF:CF_INVF + 32].unsqueeze(1).broadcast_to([128, NTT, 32]), ALU.mult, [posf, cf], [ang])

                def sin_of(shift, outs):
                    ts("dve", a2[:, :, :], ang[:, :, :], shift, None, ALU.add, None, [ang], [a2])
                    ts("dve", ki[:, :, :], a2[:, :, :], 1.0 / TWO_PI, None, ALU.mult, None, [a2], [ki])
                    E("dve", lambda e: e.tensor_copy(out=kf[:, :, :], in_=ki[:, :, :]), [ki], [kf])
                    stt(a2[:, :, :], kf[:, :, :], -C1, a2[:, :, :], ALU.mult, ALU.add, [kf, a2], [a2])
                    stt(a2[:, :, :], kf[:, :, :], -C2, a2[:, :, :], ALU.mult, ALU.add, [kf, a2], [a2])
                    ts("dve", mk[:, :, :], a2[:, :, :], math.pi, -TWO_PI, ALU.is_gt, ALU.mult, [a2], [mk])
                    tt("dve", a2[:, :, :], a2[:, :, :], mk[:, :, :], ALU.add, [a2, mk], [a2])
                    ts("dve", mk[:, :, :], a2[:, :, :], -math.pi, TWO_PI, ALU.is_lt, ALU.mult, [a2], [mk])
                    tt("dve", a2[:, :, :], a2[:, :, :], mk[:, :, :], ALU.add, [a2, mk], [a2])
                    ts("dve", a2[:, :, :], a2[:, :, :], -math.pi, math.pi, ALU.max, ALU.min, [a2], [a2])
                    for (oap, neg, ob) in outs:
                        act(oap, a2[:, :, :], AF.Sin, [a2], [ob], scale=(-1.0 if neg else 1.0))

                sin_of(0.0, [(SNt[:, :, :], False, SNt)])
                sin_of(math.pi / 2, [(CCt[:, :, :], False, CCt)])
                S.barrier()

            def ring(name, shape, dt, n=1):
                if n == 1:
                    t_ = sb(pa, name, shape, dt)
                    return [t_, t_]
                return [sb(pa, "%s%d" % (name, i), shape, dt) for i in range(n)]

            xt = ring("xt", [128, D], F32, 2)
            lnt = [(sb(pa, "st6_0", [128, 2, 6], F32), sb(pa, "mv_0", [128, 2], F32),
                    sb(pa, "sc2_0", [128, 2], F32), sb(pa, "xn_0", [128, D], F32))] * 2
            h0 = ring("h0", [128, D], F32, 2)
            h0T = ring("h0T", [128, 8, 128], BF16, 2)
            glrT = ring("glrT", [16, 128], F32)
            el = ring("el", [128, 256], F32)
            ebx = ring("ebx", [128, 3, 256], F32)
            decg = ring("decg", [128, 2], F32)
            qkg = ring("qkg", [128, 4, 256], BF16)
            kdg = ring("kdg", [128, 256], BF16)
            rA = ring("rA", [128, 256], F32)
            rB = ring("rB", [128, 256], F32)
            rot = ring("rot", [128, 256], F32)
            qkr = ring("qkr", [128, 3, 256], BF16)
            kdr = ring("kdr", [128, 256], BF16)
            vg = ring("vg", [128, 512], BF16)
            vr = ring("vr", [128, 512], BF16)
            sgg = ring("sgg", [128, 512], F32, 2)
            sgr = ring("sgr", [128, 512], F32, 2)
            TT = ring("TT", [128, 14, 128], BF16)
            tmk = ring("tmk", [128, 2, 2, 128], F32)
            STg = ring("STg", [128, 4, 128], BF16)
            STr = ring("STr", [128, 4, 128], BF16)
            Sgf = sb(pa, "Sgf", [128, 2, 128], F32)
            Srf = sb(pa, "Srf", [128, 2, 128], F32)
            Sgb = [ring("Sgb_m%d" % m, [128, 2, 128], BF16, 2) for m in range(2)]
            Srb = [ring("Srb_m%d" % m, [128, 2, 128], BF16, 2) for m in range(2)]
            TM = [sb(pa, "TM%d" % m, [128, 6, 128], BF16) for m in range(2)]
            sq = ring("sq", [128, 512], F32)
            sm = ring("sm", [128, 8, 4], F32)
            on = ring("on", [128, 512], F32)
            cat = ring("cat", [128, D], BF16)
            catT = ring("catT", [128, 8, 128], BF16)
            rr = ring("rr", [128, D], F32)
            h1 = ring("h1", [128, D], F32)
            h1b = ring("h1b", [128, D], BF16)
            h1T = ring("h1T", [128, 8, 128], F32)
            rt = ring("rt", [128, 8, 32], F32)
            rs8 = ring("rs8", [128, 3, 8], F32)
            maskb = ring("maskb", [128, 32], BF16)
            base = sb(pa, "base", [128, 32], F32)

            NROT[0] = 4
            E("dve", lambda e: e.memset(Sgf[:, :, :], 0.0), [], [Sgf])
            E("dve", lambda e: e.memset(Srf[:, :, :], 0.0), [], [Srf])
            for m in range(2):
                E("dve", lambda e, m=m: e.memset(TM[m][:, :, :], 0.0), [], [TM[m]])
                for sl in range(2):
                    E("dve", lambda e, m=m, sl=sl: e.memset(Sgb[m][sl][:, :, :], 0.0), [], [Sgb[m][sl]])
                    E("dve", lambda e, m=m, sl=sl: e.memset(Srb[m][sl][:, :, :], 0.0), [], [Srb[m][sl]])
            E("dve", lambda e: e.memset(base[:, :], 0.0), [], [base])
            scur = [0]

            TRI = cf[:, CF_TRI:CF_TRI + 128]
            SUF = cf[:, CF_SUF:CF_SUF + 128]

            def rotary(src_ap, srcT, tg, i, dst_ap, dstT, dec_col, dec_ap, decT):
                sv = src_ap.rearrange("p (h d) -> p h d", d=64)
                A, B, R = rA[i], rB[i], rot[i]
                Av = A[:, :].rearrange("p (h d) -> p h d", d=64)
                Bv = B[:, :].rearrange("p (h d) -> p h d", d=64)
                Rv = R[:, :].rearrange("p (h d) -> p h d", d=64)
                tt("dve", A[:, :].rearrange("p (h t d) -> p h t d", h=4, t=2), src_ap.rearrange("p (h t d) -> p h t d", h=4, t=2),
                   CCt[:, tg, :].unsqueeze(1).unsqueeze(1).broadcast_to([128, 4, 2, 32]), ALU.mult, [srcT, CCt], [A])
                tt("dve", Bv[:, :, 0:32], sv[:, :, 32:64], SNt[:, tg, :].unsqueeze(1).broadcast_to([128, 4, 32]),
                   ALU.mult, [srcT, SNt], [B])
                tt("dve", Bv[:, :, 32:64], sv[:, :, 0:32], SNt[:, tg, :].unsqueeze(1).broadcast_to([128, 4, 32]),
                   ALU.mult, [srcT, SNt, B], [B])
                tt("dve", Rv[:, :, 0:32], Av[:, :, 0:32], Bv[:, :, 0:32], ALU.subtract, [A, B], [R])
                tt("dve", Rv[:, :, 32:64], Av[:, :, 32:64], Bv[:, :, 32:64], ALU.add, [A, B, R], [R])
                if dst_ap is not None:
                    act(dst_ap, R[:, :], AF.Copy, [R], [dstT])
                tt("dve", dec_ap.rearrange("p (h d) -> p h d", d=64), Rv,
                   cf[:, dec_col:dec_col + 4].unsqueeze(2).broadcast_to([128, 4, 64]), ALU.mult, [R, cf], [decT])

            def tile_pass(tg, main, tl):
                i = tg % 2
                xsrc = x_own if main else x_pre
                dma("sp", xt[i][:, :], xsrc[tl * 128:(tl + 1) * 128, :], [], [xt[i]], "x%d" % i)
                layer_norm(lnt[i], xt[i][:, :], xt[i], lnp[0], lnp[1], h0[i][:, :], h0[i])
                yield "F0"
                for half in range(2):
                    bk = nf()
                    for j in range(4):
                        c = half * 4 + j
                        trp(bk[:, j * 128:(j + 1) * 128], h0[i][:, c * 128:(c + 1) * 128], identf, [h0[i], cf], [bk], sig=(j == 3))
                    act(h0T[i][:, half * 4:half * 4 + 4, :], bk[:, :].rearrange("p (a b) -> p a b", b=128), AF.Copy,
                        [bk], [h0T[i]])

                def proj(g0, n):
                    bk = nf()
                    for c in range(8):
                        mm(bk[:, 0:n], h0T[i][:, c, :], Win[:, c, g0:g0 + n], c == 0, c == 7, [h0T[i], Win], [bk])
                    return bk

                bk = nf()
                for c in range(8):
                    mm(bk[0:16, 0:128], Win[:, c, 3072:3088], h0T[i][:, c, :], c == 0, c == 7, [h0T[i], Win], [bk])
                act(glrT[i][:, :], bk[0:16, 0:128], AF.Copy, [bk], [glrT[i]])
                bz = nf()
                mm(bz[:, 0:256], glrT[i][:, :], gw[:, :], True, False, [glrT[i], gw], [bz])
                mm(bz[:, 0:256], cf[0:1, CF_ONES:CF_ONES + 128], gbr[:, :], False, True, [cf, gbr], [bz])
                act(el[i][:, :], bz[:, 0:256], AF.Exp, [bz], [el[i]], scale=-1.0)
                act(el[i][:, :], el[i][:, :], AF.Ln, [el[i]], [el[i]], bias=epsb[:, 1:2])
                bc = nf()
                if main:
                    mm(bc[:, 0:256], TRI, el[i][:, :], True, True, [cf, el[i]], [bc])
                mm(bc[:, 256:512], SUF, el[i][:, :], True, True, [cf, el[i]], [bc])
                bd = nf()
                for hp in range(2):
                    mm(bd[:, 2 * hp:2 * hp + 2], el[i][:, hp * 128:(hp + 1) * 128], cf[:, CF_N16:CF_N16 + 2], True, True,
                       [el[i], cf], [bd])
                if main:
                    act(ebx[i][:, 0, :], bc[:, 0:256], AF.Exp, [bc], [ebx[i]])
                    act(ebx[i][:, 1, :], bc[:, 0:256], AF.Exp, [bc, ebx[i]], [ebx[i]], scale=-1.0)
                act(ebx[i][:, 2, :], bc[:, 256:512], AF.Exp, [bc, ebx[i]], [ebx[i]])
                for hp in range(2):
                    act(decg[i][:, hp:hp + 1], bd[:, 2 * hp:2 * hp + 1], AF.Exp, [bd, decg[i]], [decg[i]])

                if main:
                    yield "F1a"
                bV = proj(1024, 512)
                act(vg[i][:, :], bV[:, :], AF.Copy, [bV], [vg[i]])
                bV = proj(1536, 512)
                act(vr[i][:, :], bV[:, :], AF.Copy, [bV], [vr[i]])
                if main:
                    bG = proj(2048, 512)
                    act(sgg[i][:, :], bG[:, :], AF.Silu, [bG], [sgg[i]])
                    bG = proj(2560, 512)
                    act(sgr[i][:, :], bG[:, :], AF.Silu, [bG], [sgr[i]])
                bK = proj(0, 512)
                tt("dve", kdg[i][:, :], bK[:, 0:256], ebx[i][:, 2, :], ALU.mult, [bK, ebx[i]], [kdg[i]])
                if main:
                    tt("dve", qkg[i][:, 2, :], bK[:, 0:256], ebx[i][:, 0, :], ALU.mult, [bK, ebx[i]], [qkg[i]])
                    tt("dve", qkg[i][:, 3, :], bK[:, 0:256], ebx[i][:, 1, :], ALU.mult, [bK, ebx[i], qkg[i]], [qkg[i]])
                rotary(bK[:, 256:512], bK, tg, i, (qkr[i][:, 1, :] if main else None), qkr[i], CF_DK, kdr[i][:, :], kdr[i])
                if main:
                    bQ = proj(512, 512)
                    tt("dve", qkg[i][:, 0, :], bQ[:, 0:256], ebx[i][:, 0, :], ALU.mult, [bQ, ebx[i], qkg[i]], [qkg[i]])
                    tt("dve", qkg[i][:, 1, :], bQ[:, 0:256], ebx[i][:, 1, :], ALU.mult, [bQ, ebx[i], qkg[i]], [qkg[i]])
                    rotary(bQ[:, 256:512], bQ, tg, i, qkr[i][:, 0, :], qkr[i], CF_DQ, qkr[i][:, 2, :], qkr[i])
                if main:
                    yield "F1"
                sc = scur[0]
                sn = 1 - sc
                if main and KSUB > 1:
                    srcs = []
                    for v in range(4):
                        for hp in range(2):
                            srcs.append((qkg[i][:, v, hp * 128:(hp + 1) * 128], qkg[i]))
                    for v in (0, 1, 2):
                        for hp in range(2):
                            srcs.append((qkr[i][:, v, hp * 128:(hp + 1) * 128], qkr[i]))
                    for g0 in range(0, 14, 4):
                        n = min(4, 14 - g0)
                        bk = nb()
                        for j in range(n):
                            sap, sT = srcs[g0 + j]
                            trp(bk[:, j * 128:(j + 1) * 128], sap, identb[:, :], [sT, identb], [bk], sig=(j == n - 1))
                        E("dve", lambda e, bk=bk, g0=g0, n=n: e.tensor_copy(
                            out=TT[i][:, g0:g0 + n, :], in_=bk[:, 0:n * 128].rearrange("p (a b) -> p a b", b=128)),
                          [bk, TT[i]], [TT[i]])
                        if g0 == 0 or g0 == 8:
                            nm, o0 = (4, 0) if g0 == 0 else (2, 4)
                            for m in range(2):
                                E("dve", lambda e, m=m, bk=bk, o0=o0, nm=nm: e.tensor_copy(
                                    out=TM[m][64 * m:64 * m + 64, o0:o0 + nm, :],
                                    in_=bk[64 * m:64 * m + 64, 0:nm * 128].rearrange("p (a b) -> p a b", b=128)),
                                  [bk, TM[m]], [TM[m]])
                    for hp in range(2 if (KSUB > 2 and not (KVAR & 1)) else 0):
                        bk = nf()
                        bv = bk[:, :].rearrange("p (h v i) -> p h v i", h=2, v=2)
                        for hh in range(2):
                            p0 = 64 * hh
                            mm(bv[:, hh, 0, :], TT[i][:, 6 + hp, :], TM[hh][:, 0 + hp, :], True, True,
                               [TT[i], TM[hh]], [bk])
                            mm(bv[:, hh, 1, :], TT[i][:, 4 + hp, :], TM[hh][:, 2 + hp, :], True, True,
                               [TT[i], TM[hh]], [bk])
                        m2 = cf[:, CF_LM:CF_LM + 256].rearrange("p (v i) -> p v i", v=2)
                        for hh in range(2):
                            tt("dve", tmk[i][:, hh, :, :], bv[:, hh, :, :], m2, ALU.mult, [bk, cf, tmk[i]], [tmk[i]])
                        tt("dve", STg[i][:, 2 * hp:2 * hp + 2, :], tmk[i][:, :, 0, :], tmk[i][:, :, 1, :], ALU.add,
                           [tmk[i], STg[i]], [STg[i]])
                    bk = nf()
                    for h in range(4 if (KSUB > 2 and not (KVAR & 2)) else 0):
                        hp, p0 = h // 2, 64 * (h % 2)
                        mm(bk[:, h * 128:(h + 1) * 128], TT[i][:, 10 + hp, :], TM[h % 2][:, 4 + hp, :],
                           True, True, [TT[i], TM[h % 2]], [bk])
                    if not (KVAR & 2):
                      tt("dve", STr[i][:, :, :], bk[:, :].rearrange("p (h i) -> p h i", h=4),
                       cf[:, CF_DT:CF_DT + 512].rearrange("p (h i) -> p h i", h=4), ALU.mult, [bk, cf], [STr[i]])
                    bog = pf[4]
                    for h in range(4 if KSUB > 3 else 0):
                        hp, p0 = h // 2, 64 * (h % 2)
                        mm(bog[:, h * 128:(h + 1) * 128], STg[i][:, h, :], vg[i][:, h * 128:(h + 1) * 128], True, False,
                           [STg[i], vg[i]], [bog])
                        mm(bog[:, h * 128:(h + 1) * 128], TT[i][:, 0 + hp, :], Sgb[h % 2][sc][:, hp, :],
                           False, True, [TT[i], Sgb[h % 2][sc]], [bog])
                    bor = pf[5]
                    for h in range(4 if KSUB > 3 else 0):
                        hp, p0 = h // 2, 64 * (h % 2)
                        mm(bor[:, h * 128:(h + 1) * 128], STr[i][:, h, :], vr[i][:, h * 128:(h + 1) * 128], True, False,
                           [STr[i], vr[i]], [bor])
                        mm(bor[:, h * 128:(h + 1) * 128], TT[i][:, 12 + hp, :], Srb[h % 2][sc][:, hp, :],
                           False, True, [TT[i], Srb[h % 2][sc]], [bor])
                for (kd, vv, Sf, Sb, isg) in ((kdg[i], vg[i], Sgf, Sgb, True), (kdr[i], vr[i], Srf, Srb, False)):
                    bk = nf()
                    for hp in range(2):
                        mm(bk[:, hp * 256:(hp + 1) * 256], kd[:, hp * 128:(hp + 1) * 128], vv[:, hp * 256:(hp + 1) * 256],
                           True, True, [kd, vv], [bk])
                    for hp in range(2):
                        for hh in range(2):
                            p0 = 64 * hh
                            if isg:
                                dsc, dT = decg[i][p0:p0 + 64, hp:hp + 1], decg[i]
                            else:
                                dsc, dT = cf[p0:p0 + 64, CF_DECR + hp:CF_DECR + hp + 1], cf
                            stt(Sf[p0:p0 + 64, hp, :], Sf[p0:p0 + 64, hp, :], dsc,
                                bk[p0:p0 + 64, hp * 256 + hh * 128:hp * 256 + hh * 128 + 128], ALU.mult, ALU.add,
                                [Sf, dT, bk], [Sf])
                    if (not main) and tl == NPRE - 1:
                        ts("dve", Sf[:, :, :], Sf[:, :, :], flag[:, 0:1], None, ALU.mult, None, [Sf, flag], [Sf])
                    for m in range(2):
                        act(Sb[m][sn][64 * m:64 * m + 64, :, :], Sf[64 * m:64 * m + 64, :, :], AF.Copy, [Sf], [Sb[m][sn]])
                scur[0] = sn
                if not main or KSUB <= 4:
                    return
                yield "F2"
                smv = sm[i]
                act(sq[i][:, :], bog[:, :], AF.Square, [bog], [sq[i]])
                E("dve", lambda e: e.reduce_sum(out=smv[:, 0, :], in_=sq[i][:, :].rearrange("p (h d) -> p h d", h=4),
                                                axis=AX.X), [sq[i]], [smv])
                ts("dve", smv[:, 1, :], smv[:, 0, :], 1.0 / (128.0 * 64.0), EPS, ALU.mult, ALU.add, [smv], [smv])
                act(smv[:, 1, :], smv[:, 1, :], AF.Ln, [smv], [smv])
                act(smv[:, 1, :], smv[:, 1, :], AF.Exp, [smv], [smv], scale=-0.5, bias=epsb[:, 2:3])
                tt("dve", on[i][:, :].rearrange("p (h d) -> p h d", h=4), bog[:, :].rearrange("p (h d) -> p h d", h=4),
                   smv[:, 1, :].unsqueeze(2).broadcast_to([128, 4, 128]), ALU.mult, [bog, smv], [on[i]])
                tt("dve", on[i][:, :].rearrange("p (h d) -> p h d", h=4), on[i][:, :].rearrange("p (h d) -> p h d", h=4),
                   gng[:, :].unsqueeze(1).broadcast_to([128, 4, 128]), ALU.mult, [on[i], gng], [on[i]])
                tt("dve", cat[i][:, 0:512], on[i][:, :], sgg[i][:, :], ALU.mult, [on[i], sgg[i]], [cat[i]])
                E("dve", lambda e: e.reduce_sum(out=smv[:, 2, :], in_=bor[:, :].rearrange("p (h d) -> p h d", h=4),
                                                axis=AX.X), [bor, smv], [smv])
                act(sq[i][:, :], bor[:, :], AF.Square, [bor], [sq[i]])
                E("dve", lambda e: e.reduce_sum(out=smv[:, 3, :], in_=sq[i][:, :].rearrange("p (h d) -> p h d", h=4),
                                                axis=AX.X), [sq[i], smv], [smv])
                ts("dve", smv[:, 4, :], smv[:, 2, :], 1.0 / 128.0, None, ALU.mult, None, [smv], [smv])
                tt("dve", smv[:, 5, :], smv[:, 4, :], smv[:, 4, :], ALU.mult, [smv], [smv])
                stt(smv[:, 6, :], smv[:, 3, :], 1.0 / 128.0, smv[:, 5, :], ALU.mult, ALU.subtract, [smv], [smv])
                ts("dve", smv[:, 6, :], smv[:, 6, :], 1.0 / 64.0, EPS, ALU.mult, ALU.add, [smv], [smv])
                act(smv[:, 6, :], smv[:, 6, :], AF.Ln, [smv], [smv])
                act(smv[:, 6, :], smv[:, 6, :], AF.Exp, [smv], [smv], scale=-0.5, bias=epsb[:, 2:3])
                for h in range(4):
                    ts("dve", on[i][:, h * 128:(h + 1) * 128], bor[:, h * 128:(h + 1) * 128], smv[:, 4, h:h + 1],
                       smv[:, 6, h:h + 1], ALU.subtract, ALU.mult, [bor, smv, on[i]], [on[i]])
                tt("dve", on[i][:, :], on[i][:, :], rng[:, :], ALU.mult, [on[i], rng], [on[i]])
                tt("dve", on[i][:, :], on[i][:, :], rnb[:, :], ALU.add, [on[i], rnb], [on[i]])
                tt("dve", cat[i][:, 512:1024], on[i][:, :], sgr[i][:, :], ALU.mult, [on[i], sgr[i], cat[i]], [cat[i]])
                if KSUB <= 5:
                    return
                yield "B1a"
                for half in range(2):
                    bk = nb()
                    for j in range(4):
                        c = half * 4 + j
                        trp(bk[:, j * 128:(j + 1) * 128], cat[i][:, c * 128:(c + 1) * 128], identb[:, :],
                            [cat[i], identb], [bk], sig=(j == 3))
                    E("dve", lambda e, bk=bk, half=half: e.tensor_copy(
                        out=catT[i][:, half * 4:half * 4 + 4, :], in_=bk[:, 0:512].rearrange("p (a b) -> p a b", b=128)),
                      [bk, catT[i]], [catT[i]])
                for half in range(2):
                    bk = nf()
                    for c in range(8):
                        mm(bk[:, :], catT[i][:, c, :], Wout[:, c, half * 512:(half + 1) * 512], c == 0, c == 7,
                           [catT[i], Wout], [bk])
                    stt(rr[i][:, half * 512:(half + 1) * 512], h0[i][:, half * 512:(half + 1) * 512], ALPHA, bk[:, :],
                        ALU.mult, ALU.add, [h0[i], bk, rr[i]], [rr[i]])
                layer_norm(lnt[i], rr[i][:, :], rr[i], lnp[2], lnp[3], h1[i][:, :], h1[i])
                yield "B1"
                dma("sp", h1_d[tl * 128:(tl + 1) * 128, :], h1[i][:, :], [h1[i]], [], "h1o%d" % i, accw=[h1_db])
                act(h1b[i][:, :], h1[i][:, :], AF.Copy, [h1[i]], [h1b[i]])
                if KSUB <= 6:
                    return
                for half in range(2):
                    bk = nf()
                    for j in range(4):
                        c = half * 4 + j
                        trp(bk[:, j * 128:(j + 1) * 128], h1[i][:, c * 128:(c + 1) * 128], identf, [h1[i], cf], [bk], sig=(j == 3))
                    act(h1T[i][:, half * 4:half * 4 + 4, :], bk[:, :].rearrange("p (a b) -> p a b", b=128), AF.Copy,
                        [bk, h1T[i]], [h1T[i]])
                bk = nf()
                for c in range(8):
                    mm(bk[:, 0:NE], h1T[i][:, c, :], rw[:, c, :], c == 0, False, [h1T[i], rw], [bk])
                mm(bk[:, 0:NE], cf[0:1, CF_ONES:CF_ONES + 128], rbr[:, :], False, True, [cf, rbr], [bk])
                if KSUB <= 7:
                    return
                R_, r8 = rt[i], rs8[i]
                lg, msk, ex, exm, rk, vld, d1, dsm = [R_[:, j, :] for j in range(8)]
                E("dve", lambda e: e.tensor_copy(out=lg, in_=bk[:, 0:NE]), [bk], [R_])
                E("dve", lambda e: e.max(out=r8[:, 0, :], in_=lg), [R_], [r8])
                ts("dve", msk, lg, r8[:, 0, 3:4], None, ALU.is_ge, None, [R_, r8], [R_])
                ts("dve", r8[:, 2, 0:1], r8[:, 0, 0:1], -1.0, None, ALU.mult, None, [r8], [r8])
                act(ex, lg, AF.Exp, [R_, r8], [R_], bias=r8[:, 2, 0:1])
                tt("dve", exm, ex, msk, ALU.mult, [R_], [R_])
                E("dve", lambda e: e.reduce_sum(out=r8[:, 2, 1:2], in_=exm, axis=AX.X), [R_, r8], [r8])
                E("dve", lambda e: e.reciprocal(out=r8[:, 2, 2:3], in_=r8[:, 2, 1:2]), [r8], [r8])
                E("dve", lambda e: e.tensor_copy(out=maskb[i][:, :], in_=msk), [R_], [maskb[i]])
                bk2 = nf()
                mm(bk2[:, 0:32], strictb[:, :], maskb[i][:, :], True, True, [strictb, maskb[i]], [bk2])
                mm(bk2[:, 32:64], onesb[:, :], maskb[i][:, :], True, True, [onesb, maskb[i]], [bk2])
                tt("dve", rk, bk2[:, 0:32], base[:, :], ALU.add, [bk2, base, R_], [R_])
                tt("dve", base[:, :], bk2[:, 32:64], base[:, :], ALU.add, [bk2, base], [base])
                ts("dve", vld, rk, float(CAP), None, ALU.is_lt, None, [R_], [R_])
                tt("dve", vld, vld, msk, ALU.mult, [R_], [R_])
                tt("dve", d1, rk, cf[:, CF_EOFF:CF_EOFF + 32], ALU.add, [R_, cf], [R_])
                tt("dve", dsm, d1, vld, ALU.mult, [R_], [R_])
                stt(exm, exm, r8[:, 2, 2:3], vld, ALU.mult, ALU.mult, [R_, r8], [R_])
                E("dve", lambda e: e.max(out=r8[:, 1, :], in_=dsm), [R_, r8], [r8])
                ts("dve", idx_tab[:, tl, :], r8[:, 1, 0:4], -1.0, None, ALU.add, None, [r8], [idx_b[tl]])
                for j in range(4):
                    stt(d1, dsm, r8[:, 1, j:j + 1], exm, ALU.is_equal, ALU.mult, [R_, r8], [R_])
                    E("dve", lambda e, j=j: e.reduce_sum(out=g_tab[:, tl, j:j + 1], in_=d1, axis=AX.X),
                      [R_, g_b[tl]], [g_b[tl]])
                for j in range(4 if KSTOP > 2 else 0):
                    E("pool", lambda e, j=j: e.indirect_dma_start(
                        out=xs_d[:, :], out_offset=bass.IndirectOffsetOnAxis(ap=idx_tab[:, tl, j:j + 1], axis=0),
                        in_=h1b[i][:, :], in_offset=None, bounds_check=bc_reg, oob_is_err=False),
                      [idx_b[tl], h1b[i]], [], [xs_db], dsem="sct%d" % i)

            stop_if(0)
            pgens = [tile_pass(tp, False, tp) for tp in range(NPRE)]
            if NPRE > 0:
                next(pgens[0])
            for tp in range(NPRE):
                if tp + 1 < NPRE:
                    next(pgens[tp + 1])
                for _ in pgens[tp]:
                    pass
            stop_if(1)
            gens = [tile_pass(NPRE + tm, True, tm) for tm in range(NT)]

            def step(tm_):
                if 0 <= tm_ < NT:
                    return next(gens[tm_], None)
                return None

            for _ in range(4):
                step(0)
            step(1)
            for tm in range(NT):
                step(tm + 1)
                step(tm)
                step(tm + 1)
                step(tm)
                step(tm + 1)
                step(tm + 2)
                while step(tm) is not None:
                    pass
            NROT[0] = 6
            S.barrier()
            stop_if(3)

        with ExitStack() as pbk:
            W1b = [sb(pbk, "W1b%d" % i, [128, 8, 2 * D], BF16) for i in range(2)]
            W2b = [sb(pbk, "W2b%d" % i, [128, 8, D], BF16) for i in range(2)]
            b2b = [sb(pbk, "b2b%d" % i, [1, D], BF16) for i in range(2)]
            b1T = sb(pbk, "b1T", [128, NE * 16], F32)
            XT = [sb(pbk, "XT%d" % i, [128, 8, CAP], BF16) for i in range(2)]
            AT = [sb(pbk, "AT%d" % i, [128, 8, CAP], BF16) for i in range(2)]
            NW = max(n for _, n in nch)
            tg_ = [sb(pbk, "tg%d" % i, [128, NW], F32) for i in range(2)]
            tsg = [sb(pbk, "tsg%d" % i, [128, NW], F32) for i in range(2)]
            tl_ = [sb(pbk, "tl%d" % i, [128, NW], F32) for i in range(2)]
            Ysb = [sb(pbk, "Ysb%d" % i, [128, D], F32) for i in range(2)]
            dma("sp", b1T[:, :], b1_d[:, :], [], [b1T], "c1")
            b1P = sb(pbk, "b1P", [128, NE * 16], F32)
            ts("dve", b1P[:, :], b1T[:, :], 1.0, None, ALU.add, None, [b1T], [b1P])

            def load_w(e):
                s = e % 2
                for c in range(8):
                    if c == 0:
                        dma("pool", W1b[s][:, c, :], w1_d[e, c * 128:(c + 1) * 128, :], [], [W1b[s]], "w1_%d" % s)
                    else:
                        dma("pool", W1b[s][:, c, :], w1_d[e, c * 128:(c + 1) * 128, :], [], [], "w1_%d" % s,
                            accw=[W1b[s]])
                for c in range(8):
                    if c == 0:
                        dma("pool", W2b[s][:, c, :], w2_d[e, c * 128:(c + 1) * 128, :], [], [W2b[s]], "w2_%d" % s)
                    else:
                        dma("pool", W2b[s][:, c, :], w2_d[e, c * 128:(c + 1) * 128, :], [], [], "w2_%d" % s,
                            accw=[W2b[s]])
                dma("pool", b2b[s][:, :], b2_d[e:e + 1, :], [], [b2b[s]], "b2_%d" % s)

            XR = [sb(pbk, "XR%d" % i, [128, NBLK, D], BF16) for i in range(2)]

            def load_x(e):
                s = e % 2
                for blk in range(NBLK):
                    r0 = e * CAP + blk * 128
                    if blk == 0:
                        dma("sp", XR[s][:, blk, :], xs_d[r0:r0 + 128, :], [xs_db], [XR[s]], "xr%d" % s)
                    else:
                        dma("sp", XR[s][:, blk, :], xs_d[r0:r0 + 128, :], [xs_db], [], "xr%d" % s, accw=[XR[s]])

            def transposes(e):
                s = e % 2
                for blk in range(NBLK):
                    for half in range(2):
                        bk = nb()
                        for j in range(4):
                            c = half * 4 + j
                            trp(bk[:, j * 128:(j + 1) * 128], XR[s][:, blk, c * 128:(c + 1) * 128], identb[:, :],
                                [XR[s], identb], [bk], sig=(j == 3))
                        E("dve", lambda e_, bk=bk, half=half, blk=blk, s=s: e_.tensor_copy(
                            out=XT[s][:, half * 4:half * 4 + 4, blk * 128:(blk + 1) * 128],
                            in_=bk[:, 0:512].rearrange("p (a b) -> p a b", b=128)), [bk, XT[s]], [XT[s]])

            load_w(0)
            load_x(0)
            transposes(0)
            cnt = [0]
            for e in range(NE):
                s = e % 2
                if e + 1 < NE:
                    load_w(e + 1)
                    load_x(e + 1)
                ti = 0
                for k in range(8):
                    for (n0, nsz) in nch:
                        u = ti % 2
                        ti += 1
                        bg = nf()
                        for c in range(8):
                            mm(bg[:, 0:nsz], W1b[s][:, c, k * 128:(k + 1) * 128], XT[s][:, c, n0:n0 + nsz], c == 0, c == 7,
                               [W1b[s], XT[s]], [bg])
                        bl = nf()
                        for c in range(8):
                            mm(bl[:, 0:nsz], W1b[s][:, c, D + k * 128:D + (k + 1) * 128], XT[s][:, c, n0:n0 + nsz], c == 0,
                               c == 7, [W1b[s], XT[s]], [bl])
                        ts("dve", tg_[u][:, 0:nsz], bg[:, 0:nsz], b1T[:, e * 16 + k:e * 16 + k + 1], 7.0, ALU.add, ALU.min,
                           [bg, b1T], [tg_[u]])
                        act(tsg[u][:, 0:nsz], tg_[u][:, 0:nsz], AF.Sigmoid, [tg_[u]], [tsg[u]], scale=1.702)
                        ts("dve", tl_[u][:, 0:nsz], bl[:, 0:nsz], b1P[:, e * 16 + 8 + k:e * 16 + 8 + k + 1], 8.0, ALU.add,
                           ALU.min, [bl, b1P], [tl_[u]])
                        tt("dve", tg_[u][:, 0:nsz], tg_[u][:, 0:nsz], tsg[u][:, 0:nsz], ALU.mult, [tg_[u], tsg[u]], [tg_[u]])
                        stt(AT[s][:, k, n0:n0 + nsz], tl_[u][:, 0:nsz], -6.0, tg_[u][:, 0:nsz], ALU.max, ALU.mult,
                            [tg_[u], tl_[u], AT[s]], [AT[s]])
                if e + 1 < NE:
                    transposes(e + 1)
                for blk in range(NBLK):
                    yi = cnt[0] % 2
                    cnt[0] += 1
                    r0 = e * CAP + blk * 128
                    for half in range(2):
                        bk = nf()
                        for k in range(8):
                            mm(bk[:, :], AT[s][:, k, blk * 128:(blk + 1) * 128], W2b[s][:, k, half * 512:(half + 1) * 512],
                               k == 0, False, [AT[s], W2b[s]], [bk])
                        mm(bk[:, :], onesb[0:1, :], b2b[s][0:1, half * 512:(half + 1) * 512], False, True,
                           [onesb, b2b[s]], [bk])
                        act(Ysb[yi][:, half * 512:(half + 1) * 512], bk[:, :], AF.Copy, [bk, Ysb[yi]], [Ysb[yi]])
                    dma("sp", ys_d[r0:r0 + 128, :], Ysb[yi][:, :], [Ysb[yi]], [], "yo%d" % yi, accw=[ys_db])
            S.barrier()
        stop_if(4)

        with ExitStack() as pc:
            h1t = [sb(pc, "h1t%d" % i, [128, D], F32) for i in range(2)]
            yj = [[sb(pc, "yj%d_%d" % (i, j), [128, D], F32) for j in range(4)] for i in range(2)]
            acc = [sb(pc, "acc%d" % i, [128, D], F32) for i in range(2)]
            ot = [sb(pc, "ot%d" % i, [128, D], F32) for i in range(2)]
            lnt2 = [(sb(pc, "c_st6_0", [128, 2, 6], F32), sb(pc, "c_mv_0", [128, 2], F32),
                     sb(pc, "c_sc2_0", [128, 2], F32), sb(pc, "c_xn_0", [128, D], F32))] * 2
            for i in range(2):
                for j in range(4):
                    E("dve", lambda e, i=i, j=j: e.memset(yj[i][j][:, :], 0.0), [], [yj[i][j]])
            for t in range(NT):
                i = t % 2
                dma("sp", h1t[i][:, :], h1_d[t * 128:(t + 1) * 128, :], [h1_db], [h1t[i]], "h1i%d" % i)
                for j in range(4):
                    E("pool", lambda e, i=i, j=j, t=t: e.indirect_dma_start(
                        out=yj[i][j][:, :], out_offset=None, in_=ys_d[:, :],
                        in_offset=bass.IndirectOffsetOnAxis(ap=idx_tab[:, t, j:j + 1], axis=0),
                        bounds_check=bc_reg, oob_is_err=False),
                      [idx_b[t], ys_db], [yj[i][j]], dsem="g%d_%d" % (i, j))
                act(acc[i][:, :], h1t[i][:, :], AF.Copy, [h1t[i]], [acc[i]], scale=ALPHA)
                for j in range(4):
                    stt(acc[i][:, :], yj[i][j][:, :], g_tab[:, t, j:j + 1], acc[i][:, :], ALU.mult, ALU.add,
                        [yj[i][j], g_b[t], acc[i]], [acc[i]])
                layer_norm(lnt2[i], acc[i][:, :], acc[i], ln2g, ln2b, ot[i][:, :], ot[i])
                dma("sp", out_d[t * 128:(t + 1) * 128, :], ot[i][:, :], [ot[i]], [], "oo%d" % i, accw=[out_db])
            S.barrier()
        if os.environ.get("KDEBUG"):
            print("counts", S.count, {k: v[1] for k, v in S.dsem.items()}, "ninst", S.ninst)


def make_consts(CAP):
    cf = np.zeros((128, CF_TOT), np.float64)
    j = np.arange(128)[:, None]
    i = np.arange(128)[None, :]
    cf[:, CF_IDENT:CF_IDENT + 128] = (j == i)
    cf[:, CF_TRI:CF_TRI + 128] = (j <= i) * (-1.0 / 16.0)
    cf[:, CF_SUF:CF_SUF + 128] = (j > i) * (-1.0 / 16.0)
    cf[:, CF_LM:CF_LM + 128] = (j <= i)
    cf[:, CF_LM + 128:CF_LM + 256] = (j > i) & ((j // 64) == (i // 64))
    lg = np.log1p(-np.exp2(-5.0 - np.arange(4, dtype=np.float32)).astype(np.float32)).astype(np.float32).astype(np.float64)
    for h in range(4):
        same = (j // 64) == (i // 64)
        dt = np.where(same, np.exp(lg[h] * np.abs(i - j)), np.where(j < i, np.exp(lg[h] * (i - j)), 0.0))
        cf[:, CF_DT + h * 128:CF_DT + (h + 1) * 128] = dt
        cf[:, CF_DQ + h] = np.exp(lg[h] * (np.arange(128) + 1))
        cf[:, CF_DK + h] = np.exp(lg[h] * (127 - np.arange(128)))
    for hp in range(2):
        cf[0:64, CF_DECR + hp] = np.exp(lg[2 * hp] * 128)
        cf[64:128, CF_DECR + hp] = np.exp(lg[2 * hp + 1] * 128)
    cf[:, CF_STRICT:CF_STRICT + 128] = (j < i)
    cf[:, CF_ONES:CF_ONES + 128] = 1.0
    invf = (1.0 / (np.float32(10000.0) ** np.linspace(0.0, 1.0, 32, dtype=np.float32))).astype(np.float32)
    cf[:, CF_INVF:CF_INVF + 32] = invf[None, :]
    cf[:, CF_EOFF:CF_EOFF + 32] = (np.arange(32) * CAP + 1)[None, :]
    cf[:, CF_N16:CF_N16 + 2] = -1.0 / 16.0
    return cf.astype(np.float32)


def _col_perm():
    sizes = (256, 256, 512, 512, 16, 256, 256, 512, 512)
    offs = np.cumsum((0,) + sizes)
    blk = lambda b: np.arange(offs[b], offs[b + 1])
    return np.concatenate([blk(1), blk(6), blk(0), blk(5), blk(2), blk(7), blk(3), blk(8), blk(4)])


def prepare_inputs(inputs, n_cores, NT, NPRE, CAP, seq):
    f = lambda a: np.ascontiguousarray(np.asarray(a, dtype=np.float32))
    x = f(inputs["x"])
    pos = np.asarray(inputs["positions"]).astype(np.int32)
    perm = _col_perm()
    w_in = np.ascontiguousarray(f(inputs["w_in"])[0][:, perm])
    rowp = np.concatenate([f(inputs["ln_in_g"]).reshape(-1), f(inputs["ln_in_b"]).reshape(-1),
                           f(inputs["ln1_g"]).reshape(-1), f(inputs["ln1_b"]).reshape(-1),
                           f(inputs["ln2_g"]).reshape(-1), f(inputs["ln2_b"]).reshape(-1),
                           f(inputs["gla_norm_g"]).reshape(-1), f(inputs["ret_norm_g"]).reshape(-1),
                           f(inputs["ret_norm_b"]).reshape(-1), f(inputs["router_b"]).reshape(-1)])[None, :]
    b1 = f(inputs["moe_b1"])[0]
    b1T = np.ascontiguousarray(b1.reshape(NE, 16, 128).transpose(2, 0, 1).reshape(128, NE * 16))
    shared = {
        "cf": make_consts(CAP), "w_in": w_in, "w_out": f(inputs["w_out"])[0], "gate_w": f(inputs["gla_gate_w"])[0],
        "gate_b": f(inputs["gla_gate_b"]), "rowp": np.ascontiguousarray(rowp), "router_w": f(inputs["router_w"])[0],
        "moe_w1": f(inputs["moe_w1"])[0], "moe_b1T": b1T, "moe_w2": f(inputs["moe_w2"])[0], "moe_b2": f(inputs["moe_b2"])[0],
    }
    halves = seq // (NT * 128)
    in_maps = []
    for c in range(n_cores):
        b, hf = c // halves, c % halves
        s0 = hf * NT * 128
        m = dict(shared)
        m["x_own"] = np.ascontiguousarray(x[b, s0:s0 + NT * 128])
        if NPRE > 0:
            p0 = s0 - NPRE * 128 if hf > 0 else 0
            m["x_pre"] = np.ascontiguousarray(x[b, p0:p0 + NPRE * 128])
            ppre = pos[b, p0:p0 + NPRE * 128]
        else:
            m["x_pre"] = np.zeros((128, D), np.float32)
            ppre = np.zeros((0,), np.int32)
        pall = np.concatenate([ppre, pos[b, s0:s0 + NT * 128]])
        m["pos"] = np.ascontiguousarray(pall.reshape(-1, 128).T)
        m["flag"] = np.full((128, 1), 1.0 if hf > 0 else 0.0, np.float32)
        in_maps.append(m)
    return in_maps


def run(inputs, n_cores, NT, NPRE, CAP, seq, bsz):
    nc = build_program(NT, NPRE, CAP)
    in_maps = prepare_inputs(inputs, n_cores, NT, NPRE, CAP, seq)
    res = run_bass_kernel_spmd(nc, in_maps, core_ids=list(range(n_cores)))
    halves = seq // (NT * 128)
    out = np.zeros((bsz, seq, D), np.float32)
    for c in range(n_cores):
        b, hf = c // halves, c % halves
        out[b, hf * NT * 128:(hf + 1) * NT * 128] = res.results[c]["out"]
    return out


def kernel(**inputs):
    return run(inputs, 8, 32, 32, 640, 8192, 4)
```

```python
import math
import os
from contextlib import ExitStack

import numpy as np
import concourse.bass as bass
import concourse.mybir as mybir
from concourse.bass_utils import run_bass_kernel_spmd

F32 = mybir.dt.float32
BF16 = mybir.dt.bfloat16
I32 = mybir.dt.int32
ALU = mybir.AluOpType
AF = mybir.ActivationFunctionType
AX = mybir.AxisListType

D = 1024
PW = 3088
NE = 32
ALPHA = 2.0 ** 0.25
EPS = 1e-5
TWO_PI = 2.0 * math.pi
C1 = 6.28125
C2 = TWO_PI - C1
LN8 = math.log(0.125)

CF_IDENT = 0
CF_TRI = 128
CF_SUF = 256
CF_LM = 384
CF_DT = 640
CF_STRICT = 1152
CF_ONES = 1280
CF_DQ = 1408
CF_DK = 1412
CF_DECR = 1416
CF_INVF = 1418
CF_EOFF = 1450
CF_N16 = 1482
CF_TOT = 1484


class Buf:
    __slots__ = ("wh", "wa", "r")

    def __init__(self):
        self.wh = {}
        self.wa = {}
        self.r = {}


def _mrg(d, k, s, v):
    if k not in d or d[k][1] < v:
        d[k] = (s, v)


class Sched:
    def __init__(self, nc, stack):
        self.nc = nc
        self.stack = stack
        self.eng = {"pe": nc.tensor, "dve": nc.vector, "act": nc.scalar, "pool": nc.gpsimd, "sp": nc.sync}
        self.sem = {}
        self.count = {}
        self.seen = {}
        for n in self.eng:
            self.sem[n] = stack.enter_context(nc.semaphore("s_" + n))
            self.count[n] = 0
            self.seen[n] = {}
        self.dsem = {}
        self.ninst = 0

    def _dsem(self, name):
        if name not in self.dsem:
            self.dsem[name] = [self.stack.enter_context(self.nc.semaphore("d_" + name)), 0]
        return self.dsem[name]

    def _waits(self, eng, need):
        e = self.eng[eng]
        for k, (s, v) in need.items():
            if k == eng and eng in ("pe", "sp"):
                continue
            if self.seen[eng].get(k, 0) < v:
                e.wait_ge(s, v)
                self.seen[eng][k] = v

    def emit(self, eng, fn, reads=(), writes=(), accw=(), dsem=None, sig=True):
        need = {}
        for b in reads:
            for dd in (b.wh, b.wa):
                for k, (s, v) in dd.items():
                    _mrg(need, k, s, v)
        for b in writes:
            for dd in (b.wh, b.wa, b.r):
                for k, (s, v) in dd.items():
                    _mrg(need, k, s, v)
        for b in accw:
            for dd in (b.wh, b.r):
                for k, (s, v) in dd.items():
                    _mrg(need, k, s, v)
        self._waits(eng, need)
        ins = fn(self.eng[eng])
        self.ninst += 1
        if dsem is None and not sig:
            tok = (eng, self.sem[eng], self.count[eng] + 1)
        elif dsem is None:
            self.count[eng] += 1
            ins.then_inc(self.sem[eng], 1)
            tok = (eng, self.sem[eng], self.count[eng])
        else:
            ds = self._dsem(dsem)
            ds[1] += 16
            ins.then_inc(ds[0], 16)
            tok = ("d_" + dsem, ds[0], ds[1])
        for b in reads:
            _mrg(b.r, *tok)
        for b in writes:
            b.wh = {tok[0]: (tok[1], tok[2])}
            b.wa = {}
            b.r = {}
        for b in accw:
            _mrg(b.wa, *tok)
        return tok

    def barrier(self):
        need = {}
        for n in self.eng:
            if self.count[n] > 0:
                need[n] = (self.sem[n], self.count[n])
        for name, (s, c) in self.dsem.items():
            if c > 0:
                need["d_" + name] = (s, c)
        for n in self.eng:
            nd = {k: v for k, v in need.items() if k != n}
            for k, (s, v) in nd.items():
                if self.seen[n].get(k, 0) < v:
                    self.eng[n].wait_ge(s, v)
                    self.seen[n][k] = v
            if n not in ("pe", "sp") and self.count[n] > 0:
                if self.seen[n].get(n, 0) < self.count[n]:
                    self.eng[n].wait_ge(self.sem[n], self.count[n])
                    self.seen[n][n] = self.count[n]


class _Stop(Exception):
    pass


class T:
    def __init__(self, t):
        self.t = t
        self.b = Buf()

    def __getitem__(self, k):
        return self.t[k]


def build_program(NT, NPRE, CAP):
    nc = bass.Bass("TRN2", target_bir_lowering=False)
    try:
        _build(nc, NT, NPRE, CAP)
    except _Stop:
        pass
    return nc


def _build(nc, NT, NPRE, CAP):
    NTT = NT + NPRE
    NSLOT = NE * CAP
    NBLK = CAP // 128
    nch = []
    if CAP <= 512:
        nch = [(0, CAP)]
    else:
        k = (CAP + 511) // 512
        step = ((CAP // k + 127) // 128) * 128 if (CAP // k) % 2 else CAP // k
        o = 0
        while o < CAP:
            nch.append((o, min(step, CAP - o)))
            o += step

    def din(name, shape, dt=F32):
        return nc.dram_tensor(name, shape, dt, kind="ExternalInput").ap()

    x_own = din("x_own", [NT * 128, D])
    x_pre = din("x_pre", [max(NPRE, 1) * 128, D])
    pos_in = din("pos", [128, NTT], I32)
    flag_in = din("flag", [128, 1])
    cf_in = din("cf", [128, CF_TOT])
    w_in_d = din("w_in", [D, PW])
    w_out_d = din("w_out", [D, D])
    gw_d = din("gate_w", [16, 256])
    gb_d = din("gate_b", [1, 256])
    rowp_d = din("rowp", [1, 6 * D + 128 + 512 + 512 + 32])
    rw_d = din("router_w", [D, NE])
    w1_d = din("moe_w1", [NE, D, 2 * D])
    b1_d = din("moe_b1T", [128, NE * 16])
    w2_d = din("moe_w2", [NE, D, D])
    b2_d = din("moe_b2", [NE, D])
    out_d = nc.dram_tensor("out", [NT * 128, D], F32, kind="ExternalOutput").ap()
    h1_d = nc.dram_tensor("h1_scr", [NT * 128, D], F32).ap()
    xs_d = nc.dram_tensor("xs_scr", [NSLOT, D], BF16).ap()
    ys_d = nc.dram_tensor("ys_scr", [NSLOT, D], F32).ap()
    h1_db, xs_db, ys_db, out_db = Buf(), Buf(), Buf(), Buf()

    with ExitStack() as top:
        S = Sched(nc, top)

        def sb(stack, name, shape, dt):
            return T(stack.enter_context(nc.sbuf_tensor("sb_" + name, shape, dt)))

        def ps(stack, name, shape, dt):
            return T(stack.enter_context(nc.psum_tensor(name, shape, dt)))

        def E(eng, fn, reads=(), writes=(), accw=(), dsem=None, sig=True):
            return S.emit(eng, fn, [x.b if isinstance(x, T) else x for x in reads],
                          [x.b if isinstance(x, T) else x for x in writes],
                          [x.b if isinstance(x, T) else x for x in accw], dsem, sig)

        def tt(eng, out, in0, in1, op, reads, writes):
            E(eng, lambda e: e.tensor_tensor(out=out, in0=in0, in1=in1, op=op), reads, writes)

        def ts(eng, out, in0, s1, s2, op0, op1, reads, writes):
            if op1 is None:
                E(eng, lambda e: e.tensor_scalar(out=out, in0=in0, scalar1=s1, scalar2=None, op0=op0), reads, writes)
            else:
                E(eng, lambda e: e.tensor_scalar(out=out, in0=in0, scalar1=s1, scalar2=s2, op0=op0, op1=op1),
                  reads, writes)

        def stt(out, in0, sc, in1, op0, op1, reads, writes):
            E("dve", lambda e: e.scalar_tensor_tensor(out=out, in0=in0, scalar=sc, in1=in1, op0=op0, op1=op1),
              reads, writes)

        def act(out, in_, func, reads, writes, bias=0.0, scale=1.0):
            E("act", lambda e: e.activation(out=out, in_=in_, func=func, bias=bias, scale=scale), reads, writes)

        def mm(out, lhsT, rhs, start, stop, reads, writes):
            E("pe", lambda e: e.matmul(out=out, lhsT=lhsT, rhs=rhs, start=start, stop=stop), reads, writes, sig=stop)

        def trp(out, in_, ident, reads, writes, sig=True):
            E("pe", lambda e: e.transpose(out=out, in_=in_, identity=ident), reads, writes, sig=sig)

        def dma(q, out, in_, reads, writes, sem, accw=()):
            E(q, lambda e: e.dma_start(out=out, in_=in_), reads, writes, accw, dsem=sem)

        cf = sb(top, "cf", [128, CF_TOT], F32)
        identb = sb(top, "identb", [128, 128], BF16)
        onesb = sb(top, "onesb", [128, 128], BF16)
        strictb = sb(top, "strictb", [128, 128], BF16)
        ln2g = sb(top, "ln2g", [128, D], F32)
        ln2b = sb(top, "ln2b", [128, D], F32)
        idx_tab = sb(top, "idx_tab", [128, NT, 4], I32)
        g_tab = sb(top, "g_tab", [128, NT, 4], F32)
        idx_b = [Buf() for _ in range(NT)]
        g_b = [Buf() for _ in range(NT)]
        pf = [ps(top, "pf%d" % i, [128, 512], F32) for i in range(6)]
        pb = [ps(top, "pb%d" % i, [128, 1024], BF16) for i in range(2)]
        pfi = [0]
        pbi = [0]

        def nf():
            pfi[0] += 1
            return pf[pfi[0] % NROT[0]]

        NROT = [6]

        def nb():
            pbi[0] += 1
            return pb[pbi[0] % 2]

        identf = cf[:, CF_IDENT:CF_IDENT + 128]
        bc_reg = nc.gpsimd.alloc_register("bc_reg")
        nc.gpsimd.reg_mov(bc_reg, NSLOT - 1)

        dma("sp", cf[:, :], cf_in[:, :], [], [cf], "c0")
        E("dve", lambda e: e.tensor_copy(out=identb[:, :], in_=cf[:, CF_IDENT:CF_IDENT + 128]), [cf], [identb])
        E("dve", lambda e: e.tensor_copy(out=onesb[:, :], in_=cf[:, CF_ONES:CF_ONES + 128]), [cf], [onesb])
        E("dve", lambda e: e.tensor_copy(out=strictb[:, :], in_=cf[:, CF_STRICT:CF_STRICT + 128]), [cf], [strictb])
        o_ = 4 * D
        dma("sp", ln2g[:, :], rowp_d[0:1, o_:o_ + D].broadcast_to([128, D]), [], [ln2g], "c1")
        dma("sp", ln2b[:, :], rowp_d[0:1, o_ + D:o_ + 2 * D].broadcast_to([128, D]), [], [ln2b], "c1")

        def layer_norm(stk_t, src, srcT, gT, bT, dst, dstT):
            st6, mv, sc2, xn = stk_t
            E("dve", lambda e: e.bn_stats(out=st6[:, 0, :], in_=src[:, 0:512]), [srcT], [st6])
            E("dve", lambda e: e.bn_stats(out=st6[:, 1, :], in_=src[:, 512:1024]), [srcT, st6], [st6])
            E("dve", lambda e: e.bn_aggr(out=mv[:, :], in_=st6[:, :, :].rearrange("p a b -> p (a b)")), [st6], [mv])
            act(sc2[:, 0:1], mv[:, 1:2], AF.Ln, [mv], [sc2], bias=epsb[:, 0:1])
            act(sc2[:, 0:1], sc2[:, 0:1], AF.Exp, [sc2], [sc2], scale=-0.5)
            ts("dve", sc2[:, 1:2], mv[:, 0:1], -1.0, sc2[:, 0:1], ALU.mult, ALU.mult, [mv, sc2], [sc2])
            act(xn[:, :], src, AF.Identity, [srcT, sc2], [xn], bias=sc2[:, 1:2], scale=sc2[:, 0:1])
            tt("dve", xn[:, :], xn[:, :], gT[:, :], ALU.mult, [xn, gT], [xn])
            tt("dve", dst, xn[:, :], bT[:, :], ALU.add, [xn, bT], [dstT])

        epsb = sb(top, "epsb", [128, 4], F32)
        E("dve", lambda e: e.memset(epsb[:, 0:1], EPS), [], [epsb])
        E("dve", lambda e: e.memset(epsb[:, 1:2], 1.0), [epsb], [epsb])
        E("dve", lambda e: e.memset(epsb[:, 2:3], LN8), [epsb], [epsb])

        KSTOP = int(os.environ.get("KSTOP", "9"))
        KSUB = int(os.environ.get("KSUB", "99"))
        KVAR = int(os.environ.get("KVAR", "0"))

        def stop_if(level):
            if KSTOP <= level:
                S.barrier()
                raise _Stop()

        with ExitStack() as pa:
            Win = sb(pa, "Win", [128, 8, PW], BF16)
            Wout = sb(pa, "Wout", [128, 8, D], BF16)
            gw = sb(pa, "gw", [16, 256], F32)
            gbr = sb(pa, "gbr", [1, 256], F32)
            rw = sb(pa, "rw", [128, 8, NE], F32)
            rbr = sb(pa, "rbr", [1, NE], F32)
            lnp = [sb(pa, "lnp%d" % i, [128, D], F32) for i in range(4)]
            gng = sb(pa, "gng", [128, 128], F32)
            rng = sb(pa, "rng", [128, 512], F32)
            rnb = sb(pa, "rnb", [128, 512], F32)
            flag = sb(pa, "flag", [128, 1], F32)
            posi = sb(pa, "posi", [128, NTT], I32)
            CCt = sb(pa, "CCt", [128, NTT, 32], F32)
            SNt = sb(pa, "SNt", [128, NTT, 32], F32)

            for c in range(8):
                for (a0, a1) in ((0, 1544), (1544, PW)):
                    dma("pool", Win[:, c, a0:a1], w_in_d[c * 128:(c + 1) * 128, a0:a1], [], [], "wl", accw=[Win])
            for c in range(8):
                dma("pool", Wout[:, c, :], w_out_d[c * 128:(c + 1) * 128, :], [], [], "wl", accw=[Wout])
            zt = sb(pa, "zt", [128, D], BF16)
            E("dve", lambda e: e.memset(zt[:, :], 0.0), [], [zt])
            KZ = NSLOT // 128
            xs_v = xs_d.rearrange("(p k) d -> p k d", p=128)
            zstep = min(8, KZ)
            for k0 in range(0, KZ, zstep):
                k1 = min(KZ, k0 + zstep)
                dma("pool", xs_v[:, k0:k1, :], zt[:, :].unsqueeze(1).broadcast_to([128, k1 - k0, D]), [zt], [], "zf",
                    accw=[xs_db])
            xs_db.wh, xs_db.wa = dict(xs_db.wa), {}

            dma("sp", gw[:, :], gw_d[:, :], [], [gw], "c1")
            dma("sp", gbr[:, :], gb_d[:, :], [], [gbr], "c1")
            dma("sp", rw[:, :, :], rw_d.rearrange("(c p) n -> p c n", p=128), [], [rw], "c1")
            o_ = 6 * D + 128 + 1024
            dma("sp", rbr[:, :], rowp_d[0:1, o_:o_ + NE], [], [rbr], "c1")
            for i in range(4):
                dma("sp", lnp[i][:, :], rowp_d[0:1, i * D:(i + 1) * D].broadcast_to([128, D]), [], [lnp[i]], "c1")
            o_ = 6 * D
            dma("sp", gng[:, :], rowp_d[0:1, o_:o_ + 128].broadcast_to([128, 128]), [], [gng], "c1")
            dma("sp", rng[:, :], rowp_d[0:1, o_ + 128:o_ + 640].broadcast_to([128, 512]), [], [rng], "c1")
            dma("sp", rnb[:, :], rowp_d[0:1, o_ + 640:o_ + 1152].broadcast_to([128, 512]), [], [rnb], "c1")
            dma("sp", flag[:, :], flag_in[:, :], [], [flag], "c1")
            dma("sp", posi[:, :], pos_in[:, :], [], [posi], "c1")

            with ExitStack() as pr:
                posf = sb(pr, "posf", [128, NTT], F32)
                ang = sb(pr, "ang", [128, NTT, 32], F32)
                a2 = sb(pr, "a2", [128, NTT, 32], F32)
                ki = sb(pr, "ki", [128, NTT, 32], I32)
                kf = sb(pr, "kf", [128, NTT, 32], F32)
                mk = sb(pr, "mk", [128, NTT, 32], F32)
                E("dve", lambda e: e.tensor_copy(out=posf[:, :], in_=posi[:, :]), [posi], [posf])
                tt("dve", ang[:, :, :], posf[:, :].unsqueeze(2).broadcast_to([128, NTT, 32]),
                   cf[:, CF_INVF:CF_INVF + 32].unsqueeze(1).broadcast_to([128, NTT, 32]), ALU.mult, [posf, cf], [ang])

                def sin_of(shift, outs):
                    ts("dve", a2[:, :, :], ang[:, :, :], shift, None, ALU.add, None, [ang], [a2])
                    ts("dve", ki[:, :, :], a2[:, :, :], 1.0 / TWO_PI, None, ALU.mult, None, [a2], [ki])
                    E("dve", lambda e: e.tensor_copy(out=kf[:, :, :], in_=ki[:, :, :]), [ki], [kf])
                    stt(a2[:, :, :], kf[:, :, :], -C1, a2[:, :, :], ALU.mult, ALU.add, [kf, a2], [a2])
                    stt(a2[:, :, :], kf[:, :, :], -C2, a2[:, :, :], ALU.mult, ALU.add, [kf, a2], [a2])
                    ts("dve", mk[:, :, :], a2[:, :, :], math.pi, -TWO_PI, ALU.is_gt, ALU.mult, [a2], [mk])
                    tt("dve", a2[:, :, :], a2[:, :, :], mk[:, :, :], ALU.add, [a2, mk], [a2])
                    ts("dve", mk[:, :, :], a2[:, :, :], -math.pi, TWO_PI, ALU.is_lt, ALU.mult, [a2], [mk])
                    tt("dve", a2[:, :, :], a2[:, :, :], mk[:, :, :], ALU.add, [a2, mk], [a2])
                    ts("dve", a2[:, :, :], a2[:, :, :], -math.pi, math.pi, ALU.max, ALU.min, [a2], [a2])
                    for (oap, neg, ob) in outs:
                        act(oap, a2[:, :, :], AF.Sin, [a2], [ob], scale=(-1.0 if neg else 1.0))

                sin_of(0.0, [(SNt[:, :, :], False, SNt)])
                sin_of(math.pi / 2, [(CCt[:, :, :], False, CCt)])
                S.barrier()

            def ring(name, shape, dt, n=1):
                if n == 1:
                    t_ = sb(pa, name, shape, dt)
                    return [t_, t_]
                return [sb(pa, "%s%d" % (name, i), shape, dt) for i in range(n)]

            xt = ring("xt", [128, D], F32, 2)
            lnt = [(sb(pa, "st6_0", [128, 2, 6], F32), sb(pa, "mv_0", [128, 2], F32),
                    sb(pa, "sc2_0", [128, 2], F32), sb(pa, "xn_0", [128, D], F32))] * 2
            h0 = ring("h0", [128, D], F32, 2)
            h0T = ring("h0T", [128, 8, 128], BF16, 2)
            glrT = ring("glrT", [16, 128], F32)
            el = ring("el", [128, 256], F32)
            ebx = ring("ebx", [128, 3, 256], F32)
            decg = ring("decg", [128, 2], F32)
            qkg = ring("qkg", [128, 4, 256], BF16)
            kdg = ring("kdg", [128, 256], BF16)
            rA = ring("rA", [128, 256], F32)
            rB = ring("rB", [128, 256], F32)
            rot = ring("rot", [128, 256], F32)
            qkr = ring("qkr", [128, 3, 256], BF16)
            kdr = ring("kdr", [128, 256], BF16)
            vg = ring("vg", [128, 512], BF16)
            vr = ring("vr", [128, 512], BF16)
            sgg = ring("sgg", [128, 512], F32, 2)
            sgr = ring("sgr", [128, 512], F32, 2)
            TT = ring("TT", [128, 14, 128], BF16)
            tmk = ring("tmk", [128, 2, 2, 128], F32)
            STg = ring("STg", [128, 4, 128], BF16)
            STr = ring("STr", [128, 4, 128], BF16)
            Sgf = sb(pa, "Sgf", [128, 2, 128], F32)
            Srf = sb(pa, "Srf", [128, 2, 128], F32)
            Sgb = [ring("Sgb_m%d" % m, [128, 2, 128], BF16, 2) for m in range(2)]
            Srb = [ring("Srb_m%d" % m, [128, 2, 128], BF16, 2) for m in range(2)]
            TM = [sb(pa, "TM%d" % m, [128, 6, 128], BF16) for m in range(2)]
            sq = ring("sq", [128, 512], F32)
            sm = ring("sm", [128, 8, 4], F32)
            on = ring("on", [128, 512], F32)
            cat = ring("cat", [128, D], BF16)
            catT = ring("catT", [128, 8, 128], BF16)
            rr = ring("rr", [128, D], F32)
            h1 = ring("h1", [128, D], F32)
            h1b = ring("h1b", [128, D], BF16)
            h1T = ring("h1T", [128, 8, 128], F32)
            rt = ring("rt", [128, 8, 32], F32)
            rs8 = ring("rs8", [128, 3, 8], F32)
            maskb = ring("maskb", [128, 32], BF16)
            base = sb(pa, "base", [128, 32], F32)

            NROT[0] = 4
            E("dve", lambda e: e.memset(Sgf[:, :, :], 0.0), [], [Sgf])
            E("dve", lambda e: e.memset(Srf[:, :, :], 0.0), [], [Srf])
            for m in range(2):
                E("dve", lambda e, m=m: e.memset(TM[m][:, :, :], 0.0), [], [TM[m]])
                for sl in range(2):
                    E("dve", lambda e, m=m, sl=sl: e.memset(Sgb[m][sl][:, :, :], 0.0), [], [Sgb[m][sl]])
                    E("dve", lambda e, m=m, sl=sl: e.memset(Srb[m][sl][:, :, :], 0.0), [], [Srb[m][sl]])
            E("dve", lambda e: e.memset(base[:, :], 0.0), [], [base])
            scur = [0]

            TRI = cf[:, CF_TRI:CF_TRI + 128]
            SUF = cf[:, CF_SUF:CF_SUF + 128]

            def rotary(src_ap, srcT, tg, i, dst_ap, dstT, dec_col, dec_ap, decT):
                sv = src_ap.rearrange("p (h d) -> p h d", d=64)
                A, B, R = rA[i], rB[i], rot[i]
                Av = A[:, :].rearrange("p (h d) -> p h d", d=64)
                Bv = B[:, :].rearrange("p (h d) -> p h d", d=64)
                Rv = R[:, :].rearrange("p (h d) -> p h d", d=64)
                tt("dve", A[:, :].rearrange("p (h t d) -> p h t d", h=4, t=2), src_ap.rearrange("p (h t d) -> p h t d", h=4, t=2),
                   CCt[:, tg, :].unsqueeze(1).unsqueeze(1).broadcast_to([128, 4, 2, 32]), ALU.mult, [srcT, CCt], [A])
                tt("dve", Bv[:, :, 0:32], sv[:, :, 32:64], SNt[:, tg, :].unsqueeze(1).broadcast_to([128, 4, 32]),
                   ALU.mult, [srcT, SNt], [B])
                tt("dve", Bv[:, :, 32:64], sv[:, :, 0:32], SNt[:, tg, :].unsqueeze(1).broadcast_to([128, 4, 32]),
                   ALU.mult, [srcT, SNt, B], [B])
                tt("dve", Rv[:, :, 0:32], Av[:, :, 0:32], Bv[:, :, 0:32], ALU.subtract, [A, B], [R])
                tt("dve", Rv[:, :, 32:64], Av[:, :, 32:64], Bv[:, :, 32:64], ALU.add, [A, B, R], [R])
                if dst_ap is not None:
                    act(dst_ap, R[:, :], AF.Copy, [R], [dstT])
                tt("dve", dec_ap.rearrange("p (h d) -> p h d", d=64), Rv,
                   cf[:, dec_col:dec_col + 4].unsqueeze(2).broadcast_to([128, 4, 64]), ALU.mult, [R, cf], [decT])

            def tile_pass(tg, main, tl):
                i = tg % 2
                xsrc = x_own if main else x_pre
                dma("sp", xt[i][:, :], xsrc[tl * 128:(tl + 1) * 128, :], [], [xt[i]], "x%d" % i)
                layer_norm(lnt[i], xt[i][:, :], xt[i], lnp[0], lnp[1], h0[i][:, :], h0[i])
                yield "F0"
                for half in range(2):
                    bk = nf()
                    for j in range(4):
                        c = half * 4 + j
                        trp(bk[:, j * 128:(j + 1) * 128], h0[i][:, c * 128:(c + 1) * 128], identf, [h0[i], cf], [bk], sig=(j == 3))
                    act(h0T[i][:, half * 4:half * 4 + 4, :], bk[:, :].rearrange("p (a b) -> p a b", b=128), AF.Copy,
                        [bk], [h0T[i]])

                def proj(g0, n):
                    bk = nf()
                    for c in range(8):
                        mm(bk[:, 0:n], h0T[i][:, c, :], Win[:, c, g0:g0 + n], c == 0, c == 7, [h0T[i], Win], [bk])
                    return bk

                bk = nf()
                for c in range(8):
                    mm(bk[0:16, 0:128], Win[:, c, 3072:3088], h0T[i][:, c, :], c == 0, c == 7, [h0T[i], Win], [bk])
                act(glrT[i][:, :], bk[0:16, 0:128], AF.Copy, [bk], [glrT[i]])
                bz = nf()
                mm(bz[:, 0:256], glrT[i][:, :], gw[:, :], True, False, [glrT[i], gw], [bz])
                mm(bz[:, 0:256], cf[0:1, CF_ONES:CF_ONES + 128], gbr[:, :], False, True, [cf, gbr], [bz])
                act(el[i][:, :], bz[:, 0:256], AF.Exp, [bz], [el[i]], scale=-1.0)
                act(el[i][:, :], el[i][:, :], AF.Ln, [el[i]], [el[i]], bias=epsb[:, 1:2])
                bc = nf()
                if main:
                    mm(bc[:, 0:256], TRI, el[i][:, :], True, True, [cf, el[i]], [bc])
                mm(bc[:, 256:512], SUF, el[i][:, :], True, True, [cf, el[i]], [bc])
                bd = nf()
                for hp in range(2):
                    mm(bd[:, 2 * hp:2 * hp + 2], el[i][:, hp * 128:(hp + 1) * 128], cf[:, CF_N16:CF_N16 + 2], True, True,
                       [el[i], cf], [bd])
                if main:
                    act(ebx[i][:, 0, :], bc[:, 0:256], AF.Exp, [bc], [ebx[i]])
                    act(ebx[i][:, 1, :], bc[:, 0:256], AF.Exp, [bc, ebx[i]], [ebx[i]], scale=-1.0)
                act(ebx[i][:, 2, :], bc[:, 256:512], AF.Exp, [bc, ebx[i]], [ebx[i]])
                for hp in range(2):
                    act(decg[i][:, hp:hp + 1], bd[:, 2 * hp:2 * hp + 1], AF.Exp, [bd, decg[i]], [decg[i]])

                if main:
                    yield "F1a"
                bV = proj(1024, 512)
                act(vg[i][:, :], bV[:, :], AF.Copy, [bV], [vg[i]])
                bV = proj(1536, 512)
                act(vr[i][:, :], bV[:, :], AF.Copy, [bV], [vr[i]])
                if main:
                    bG = proj(2048, 512)
                    act(sgg[i][:, :], bG[:, :], AF.Silu, [bG], [sgg[i]])
                    bG = proj(2560, 512)
                    act(sgr[i][:, :], bG[:, :], AF.Silu, [bG], [sgr[i]])
                bK = proj(0, 512)
                tt("dve", kdg[i][:, :], bK[:, 0:256], ebx[i][:, 2, :], ALU.mult, [bK, ebx[i]], [kdg[i]])
                if main:
                    tt("dve", qkg[i][:, 2, :], bK[:, 0:256], ebx[i][:, 0, :], ALU.mult, [bK, ebx[i]], [qkg[i]])
                    tt("dve", qkg[i][:, 3, :], bK[:, 0:256], ebx[i][:, 1, :], ALU.mult, [bK, ebx[i], qkg[i]], [qkg[i]])
                rotary(bK[:, 256:512], bK, tg, i, (qkr[i][:, 1, :] if main else None), qkr[i], CF_DK, kdr[i][:, :], kdr[i])
                if main:
                    bQ = proj(512, 512)
                    tt("dve", qkg[i][:, 0, :], bQ[:, 0:256], ebx[i][:, 0, :], ALU.mult, [bQ, ebx[i], qkg[i]], [qkg[i]])
                    tt("dve", qkg[i][:, 1, :], bQ[:, 0:256], ebx[i][:, 1, :], ALU.mult, [bQ, ebx[i], qkg[i]], [qkg[i]])
                    rotary(bQ[:, 256:512], bQ, tg, i, qkr[i][:, 0, :], qkr[i], CF_DQ, qkr[i][:, 2, :], qkr[i])
                if main:
                    yield "F1"
                sc = scur[0]
                sn = 1 - sc
                if main and KSUB > 1:
                    srcs = []
                    for v in range(4):
                        for hp in range(2):
                            srcs.append((qkg[i][:, v, hp * 128:(hp + 1) * 128], qkg[i]))
                    for v in (0, 1, 2):
                        for hp in range(2):
                            srcs.append((qkr[i][:, v, hp * 128:(hp + 1) * 128], qkr[i]))
                    for g0 in range(0, 14, 4):
                        n = min(4, 14 - g0)
                        bk = nb()
                        for j in range(n):
                            sap, sT = srcs[g0 + j]
                            trp(bk[:, j * 128:(j + 1) * 128], sap, identb[:, :], [sT, identb], [bk], sig=(j == n - 1))
                        E("dve", lambda e, bk=bk, g0=g0, n=n: e.tensor_copy(
                            out=TT[i][:, g0:g0 + n, :], in_=bk[:, 0:n * 128].rearrange("p (a b) -> p a b", b=128)),
                          [bk, TT[i]], [TT[i]])
                        if g0 == 0 or g0 == 8:
                            nm, o0 = (4, 0) if g0 == 0 else (2, 4)
                            for m in range(2):
                                E("dve", lambda e, m=m, bk=bk, o0=o0, nm=nm: e.tensor_copy(
                                    out=TM[m][64 * m:64 * m + 64, o0:o0 + nm, :],
                                    in_=bk[64 * m:64 * m + 64, 0:nm * 128].rearrange("p (a b) -> p a b", b=128)),
                                  [bk, TM[m]], [TM[m]])
                    for hp in range(2 if (KSUB > 2 and not (KVAR & 1)) else 0):
                        bk = nf()
                        bv = bk[:, :].rearrange("p (h v i) -> p h v i", h=2, v=2)
                        for hh in range(2):
                            p0 = 64 * hh
                            mm(bv[:, hh, 0, :], TT[i][:, 6 + hp, :], TM[hh][:, 0 + hp, :], True, True,
                               [TT[i], TM[hh]], [bk])
                            mm(bv[:, hh, 1, :], TT[i][:, 4 + hp, :], TM[hh][:, 2 + hp, :], True, True,
                               [TT[i], TM[hh]], [bk])
                        m2 = cf[:, CF_LM:CF_LM + 256].rearrange("p (v i) -> p v i", v=2)
                        for hh in range(2):
                            tt("dve", tmk[i][:, hh, :, :], bv[:, hh, :, :], m2, ALU.mult, [bk, cf, tmk[i]], [tmk[i]])
                        tt("dve", STg[i][:, 2 * hp:2 * hp + 2, :], tmk[i][:, :, 0, :], tmk[i][:, :, 1, :], ALU.add,
                           [tmk[i], STg[i]], [STg[i]])
                    bk = nf()
                    for h in range(4 if (KSUB > 2 and not (KVAR & 2)) else 0):
                        hp, p0 = h // 2, 64 * (h % 2)
                        mm(bk[:, h * 128:(h + 1) * 128], TT[i][:, 10 + hp, :], TM[h % 2][:, 4 + hp, :],
                           True, True, [TT[i], TM[h % 2]], [bk])
                    if not (KVAR & 2):
                      tt("dve", STr[i][:, :, :], bk[:, :].rearrange("p (h i) -> p h i", h=4),
                       cf[:, CF_DT:CF_DT + 512].rearrange("p (h i) -> p h i", h=4), ALU.mult, [bk, cf], [STr[i]])
                    bog = pf[4]
                    for h in range(4 if KSUB > 3 else 0):
                        hp, p0 = h // 2, 64 * (h % 2)
                        mm(bog[:, h * 128:(h + 1) * 128], STg[i][:, h, :], vg[i][:, h * 128:(h + 1) * 128], True, False,
                           [STg[i], vg[i]], [bog])
                        mm(bog[:, h * 128:(h + 1) * 128], TT[i][:, 0 + hp, :], Sgb[h % 2][sc][:, hp, :],
                           False, True, [TT[i], Sgb[h % 2][sc]], [bog])
                    bor = pf[5]
                    for h in range(4 if KSUB > 3 else 0):
                        hp, p0 = h // 2, 64 * (h % 2)
                        mm(bor[:, h * 128:(h + 1) * 128], STr[i][:, h, :], vr[i][:, h * 128:(h + 1) * 128], True, False,
                           [STr[i], vr[i]], [bor])
                        mm(bor[:, h * 128:(h + 1) * 128], TT[i][:, 12 + hp, :], Srb[h % 2][sc][:, hp, :],
                           False, True, [TT[i], Srb[h % 2][sc]], [bor])
                for (kd, vv, Sf, Sb, isg) in ((kdg[i], vg[i], Sgf, Sgb, True), (kdr[i], vr[i], Srf, Srb, False)):
                    bk = nf()
                    for hp in range(2):
                        mm(bk[:, hp * 256:(hp + 1) * 256], kd[:, hp * 128:(hp + 1) * 128], vv[:, hp * 256:(hp + 1) * 256],
                           True, True, [kd, vv], [bk])
                    for hp in range(2):
                        for hh in range(2):
                            p0 = 64 * hh
                            if isg:
                                dsc, dT = decg[i][p0:p0 + 64, hp:hp + 1], decg[i]
                            else:
                                dsc, dT = cf[p0:p0 + 64, CF_DECR + hp:CF_DECR + hp + 1], cf
                            stt(Sf[p0:p0 + 64, hp, :], Sf[p0:p0 + 64, hp, :], dsc,
                                bk[p0:p0 + 64, hp * 256 + hh * 128:hp * 256 + hh * 128 + 128], ALU.mult, ALU.add,
                                [Sf, dT, bk], [Sf])
                    if (not main) and tl == NPRE - 1:
                        ts("dve", Sf[:, :, :], Sf[:, :, :], flag[:, 0:1], None, ALU.mult, None, [Sf, flag], [Sf])
                    for m in range(2):
                        act(Sb[m][sn][64 * m:64 * m + 64, :, :], Sf[64 * m:64 * m + 64, :, :], AF.Copy, [Sf], [Sb[m][sn]])
                scur[0] = sn
                if not main or KSUB <= 4:
                    return
                yield "F2"
                smv = sm[i]
                act(sq[i][:, :], bog[:, :], AF.Square, [bog], [sq[i]])
                E("dve", lambda e: e.reduce_sum(out=smv[:, 0, :], in_=sq[i][:, :].rearrange("p (h d) -> p h d", h=4),
                                                axis=AX.X), [sq[i]], [smv])
                ts("dve", smv[:, 1, :], smv[:, 0, :], 1.0 / (128.0 * 64.0), EPS, ALU.mult, ALU.add, [smv], [smv])
                act(smv[:, 1, :], smv[:, 1, :], AF.Ln, [smv], [smv])
                act(smv[:, 1, :], smv[:, 1, :], AF.Exp, [smv], [smv], scale=-0.5, bias=epsb[:, 2:3])
                tt("dve", on[i][:, :].rearrange("p (h d) -> p h d", h=4), bog[:, :].rearrange("p (h d) -> p h d", h=4),
                   smv[:, 1, :].unsqueeze(2).broadcast_to([128, 4, 128]), ALU.mult, [bog, smv], [on[i]])
                tt("dve", on[i][:, :].rearrange("p (h d) -> p h d", h=4), on[i][:, :].rearrange("p (h d) -> p h d", h=4),
                   gng[:, :].unsqueeze(1).broadcast_to([128, 4, 128]), ALU.mult, [on[i], gng], [on[i]])
                tt("dve", cat[i][:, 0:512], on[i][:, :], sgg[i][:, :], ALU.mult, [on[i], sgg[i]], [cat[i]])
                E("dve", lambda e: e.reduce_sum(out=smv[:, 2, :], in_=bor[:, :].rearrange("p (h d) -> p h d", h=4),
                                                axis=AX.X), [bor, smv], [smv])
                act(sq[i][:, :], bor[:, :], AF.Square, [bor], [sq[i]])
                E("dve", lambda e: e.reduce_sum(out=smv[:, 3, :], in_=sq[i][:, :].rearrange("p (h d) -> p h d", h=4),
                                                axis=AX.X), [sq[i], smv], [smv])
                ts("dve", smv[:, 4, :], smv[:, 2, :], 1.0 / 128.0, None, ALU.mult, None, [smv], [smv])
                tt("dve", smv[:, 5, :], smv[:, 4, :], smv[:, 4, :], ALU.mult, [smv], [smv])
                stt(smv[:, 6, :], smv[:, 3, :], 1.0 / 128.0, smv[:, 5, :], ALU.mult, ALU.subtract, [smv], [smv])
                ts("dve", smv[:, 6, :], smv[:, 6, :], 1.0 / 64.0, EPS, ALU.mult, ALU.add, [smv], [smv])
                act(smv[:, 6, :], smv[:, 6, :], AF.Ln, [smv], [smv])
                act(smv[:, 6, :], smv[:, 6, :], AF.Exp, [smv], [smv], scale=-0.5, bias=epsb[:, 2:3])
                for h in range(4):
                    ts("dve", on[i][:, h * 128:(h + 1) * 128], bor[:, h * 128:(h + 1) * 128], smv[:, 4, h:h + 1],
                       smv[:, 6, h:h + 1], ALU.subtract, ALU.mult, [bor, smv, on[i]], [on[i]])
                tt("dve", on[i][:, :], on[i][:, :], rng[:, :], ALU.mult, [on[i], rng], [on[i]])
                tt("dve", on[i][:, :], on[i][:, :], rnb[:, :], ALU.add, [on[i], rnb], [on[i]])
                tt("dve", cat[i][:, 512:1024], on[i][:, :], sgr[i][:, :], ALU.mult, [on[i], sgr[i], cat[i]], [cat[i]])
                if KSUB <= 5:
                    return
                yield "B1a"
                for half in range(2):
                    bk = nb()
                    for j in range(4):
                        c = half * 4 + j
                        trp(bk[:, j * 128:(j + 1) * 128], cat[i][:, c * 128:(c + 1) * 128], identb[:, :],
                            [cat[i], identb], [bk], sig=(j == 3))
                    E("dve", lambda e, bk=bk, half=half: e.tensor_copy(
                        out=catT[i][:, half * 4:half * 4 + 4, :], in_=bk[:, 0:512].rearrange("p (a b) -> p a b", b=128)),
                      [bk, catT[i]], [catT[i]])
                for half in range(2):
                    bk = nf()
                    for c in range(8):
                        mm(bk[:, :], catT[i][:, c, :], Wout[:, c, half * 512:(half + 1) * 512], c == 0, c == 7,
                           [catT[i], Wout], [bk])
                    stt(rr[i][:, half * 512:(half + 1) * 512], h0[i][:, half * 512:(half + 1) * 512], ALPHA, bk[:, :],
                        ALU.mult, ALU.add, [h0[i], bk, rr[i]], [rr[i]])
                layer_norm(lnt[i], rr[i][:, :], rr[i], lnp[2], lnp[3], h1[i][:, :], h1[i])
                yield "B1"
                dma("sp", h1_d[tl * 128:(tl + 1) * 128, :], h1[i][:, :], [h1[i]], [], "h1o%d" % i, accw=[h1_db])
                act(h1b[i][:, :], h1[i][:, :], AF.Copy, [h1[i]], [h1b[i]])
                if KSUB <= 6:
                    return
                for half in range(2):
                    bk = nf()
                    for j in range(4):
                        c = half * 4 + j
                        trp(bk[:, j * 128:(j + 1) * 128], h1[i][:, c * 128:(c + 1) * 128], identf, [h1[i], cf], [bk], sig=(j == 3))
                    act(h1T[i][:, half * 4:half * 4 + 4, :], bk[:, :].rearrange("p (a b) -> p a b", b=128), AF.Copy,
                        [bk, h1T[i]], [h1T[i]])
                bk = nf()
                for c in range(8):
                    mm(bk[:, 0:NE], h1T[i][:, c, :], rw[:, c, :], c == 0, False, [h1T[i], rw], [bk])
                mm(bk[:, 0:NE], cf[0:1, CF_ONES:CF_ONES + 128], rbr[:, :], False, True, [cf, rbr], [bk])
                if KSUB <= 7:
                    return
                R_, r8 = rt[i], rs8[i]
                lg, msk, ex, exm, rk, vld, d1, dsm = [R_[:, j, :] for j in range(8)]
                E("dve", lambda e: e.tensor_copy(out=lg, in_=bk[:, 0:NE]), [bk], [R_])
                E("dve", lambda e: e.max(out=r8[:, 0, :], in_=lg), [R_], [r8])
                ts("dve", msk, lg, r8[:, 0, 3:4], None, ALU.is_ge, None, [R_, r8], [R_])
                ts("dve", r8[:, 2, 0:1], r8[:, 0, 0:1], -1.0, None, ALU.mult, None, [r8], [r8])
                act(ex, lg, AF.Exp, [R_, r8], [R_], bias=r8[:, 2, 0:1])
                tt("dve", exm, ex, msk, ALU.mult, [R_], [R_])
                E("dve", lambda e: e.reduce_sum(out=r8[:, 2, 1:2], in_=exm, axis=AX.X), [R_, r8], [r8])
                E("dve", lambda e: e.reciprocal(out=r8[:, 2, 2:3], in_=r8[:, 2, 1:2]), [r8], [r8])
                E("dve", lambda e: e.tensor_copy(out=maskb[i][:, :], in_=msk), [R_], [maskb[i]])
                bk2 = nf()
                mm(bk2[:, 0:32], strictb[:, :], maskb[i][:, :], True, True, [strictb, maskb[i]], [bk2])
                mm(bk2[:, 32:64], onesb[:, :], maskb[i][:, :], True, True, [onesb, maskb[i]], [bk2])
                tt("dve", rk, bk2[:, 0:32], base[:, :], ALU.add, [bk2, base, R_], [R_])
                tt("dve", base[:, :], bk2[:, 32:64], base[:, :], ALU.add, [bk2, base], [base])
                ts("dve", vld, rk, float(CAP), None, ALU.is_lt, None, [R_], [R_])
                tt("dve", vld, vld, msk, ALU.mult, [R_], [R_])
                tt("dve", d1, rk, cf[:, CF_EOFF:CF_EOFF + 32], ALU.add, [R_, cf], [R_])
                tt("dve", dsm, d1, vld, ALU.mult, [R_], [R_])
                stt(exm, exm, r8[:, 2, 2:3], vld, ALU.mult, ALU.mult, [R_, r8], [R_])
                E("dve", lambda e: e.max(out=r8[:, 1, :], in_=dsm), [R_, r8], [r8])
                ts("dve", idx_tab[:, tl, :], r8[:, 1, 0:4], -1.0, None, ALU.add, None, [r8], [idx_b[tl]])
                for j in range(4):
                    stt(d1, dsm, r8[:, 1, j:j + 1], exm, ALU.is_equal, ALU.mult, [R_, r8], [R_])
                    E("dve", lambda e, j=j: e.reduce_sum(out=g_tab[:, tl, j:j + 1], in_=d1, axis=AX.X),
                      [R_, g_b[tl]], [g_b[tl]])
                for j in range(4 if KSTOP > 2 else 0):
                    E("pool", lambda e, j=j: e.indirect_dma_start(
                        out=xs_d[:, :], out_offset=bass.IndirectOffsetOnAxis(ap=idx_tab[:, tl, j:j + 1], axis=0),
                        in_=h1b[i][:, :], in_offset=None, bounds_check=bc_reg, oob_is_err=False),
                      [idx_b[tl], h1b[i]], [], [xs_db], dsem="sct%d" % i)

            stop_if(0)
            pgens = [tile_pass(tp, False, tp) for tp in range(NPRE)]
            if NPRE > 0:
                next(pgens[0])
            for tp in range(NPRE):
                if tp + 1 < NPRE:
                    next(pgens[tp + 1])
                for _ in pgens[tp]:
                    pass
            stop_if(1)
            gens = [tile_pass(NPRE + tm, True, tm) for tm in range(NT)]

            def step(tm_):
                if 0 <= tm_ < NT:
                    return next(gens[tm_], None)
                return None

            for _ in range(4):
                step(0)
            step(1)
            for tm in range(NT):
                step(tm + 1)
                step(tm)
                step(tm + 1)
                step(tm)
                step(tm + 1)
                step(tm + 2)
                while step(tm) is not None:
                    pass
            NROT[0] = 6
            S.barrier()
            stop_if(3)

        with ExitStack() as pbk:
            W1b = [sb(pbk, "W1b%d" % i, [128, 8, 2 * D], BF16) for i in range(2)]
            W2b = [sb(pbk, "W2b%d" % i, [128, 8, D], BF16) for i in range(2)]
            b2b = [sb(pbk, "b2b%d" % i, [1, D], BF16) for i in range(2)]
            b1T = sb(pbk, "b1T", [128, NE * 16], F32)
            XT = [sb(pbk, "XT%d" % i, [128, 8, CAP], BF16) for i in range(2)]
            AT = [sb(pbk, "AT%d" % i, [128, 8, CAP], BF16) for i in range(2)]
            NW = max(n for _, n in nch)
            tg_ = [sb(pbk, "tg%d" % i, [128, NW], F32) for i in range(2)]
            tsg = [sb(pbk, "tsg%d" % i, [128, NW], F32) for i in range(2)]
            tl_ = [sb(pbk, "tl%d" % i, [128, NW], F32) for i in range(2)]
            Ysb = [sb(pbk, "Ysb%d" % i, [128, D], F32) for i in range(2)]
            dma("sp", b1T[:, :], b1_d[:, :], [], [b1T], "c1")
            b1P = sb(pbk, "b1P", [128, NE * 16], F32)
            ts("dve", b1P[:, :], b1T[:, :], 1.0, None, ALU.add, None, [b1T], [b1P])

            def load_w(e):
                s = e % 2
                for c in range(8):
                    if c == 0:
                        dma("pool", W1b[s][:, c, :], w1_d[e, c * 128:(c + 1) * 128, :], [], [W1b[s]], "w1_%d" % s)
                    else:
                        dma("pool", W1b[s][:, c, :], w1_d[e, c * 128:(c + 1) * 128, :], [], [], "w1_%d" % s,
                            accw=[W1b[s]])
                for c in range(8):
                    if c == 0:
                        dma("pool", W2b[s][:, c, :], w2_d[e, c * 128:(c + 1) * 128, :], [], [W2b[s]], "w2_%d" % s)
                    else:
                        dma("pool", W2b[s][:, c, :], w2_d[e, c * 128:(c + 1) * 128, :], [], [], "w2_%d" % s,
                            accw=[W2b[s]])
                dma("pool", b2b[s][:, :], b2_d[e:e + 1, :], [], [b2b[s]], "b2_%d" % s)

            XR = [sb(pbk, "XR%d" % i, [128, NBLK, D], BF16) for i in range(2)]

            def load_x(e):
                s = e % 2
                for blk in range(NBLK):
                    r0 = e * CAP + blk * 128
                    if blk == 0:
                        dma("sp", XR[s][:, blk, :], xs_d[r0:r0 + 128, :], [xs_db], [XR[s]], "xr%d" % s)
                    else:
                        dma("sp", XR[s][:, blk, :], xs_d[r0:r0 + 128, :], [xs_db], [], "xr%d" % s, accw=[XR[s]])

            def transposes(e):
                s = e % 2
                for blk in range(NBLK):
                    for half in range(2):
                        bk = nb()
                        for j in range(4):
                            c = half * 4 + j
                            trp(bk[:, j * 128:(j + 1) * 128], XR[s][:, blk, c * 128:(c + 1) * 128], identb[:, :],
                                [XR[s], identb], [bk], sig=(j == 3))
                        E("dve", lambda e_, bk=bk, half=half, blk=blk, s=s: e_.tensor_copy(
                            out=XT[s][:, half * 4:half * 4 + 4, blk * 128:(blk + 1) * 128],
                            in_=bk[:, 0:512].rearrange("p (a b) -> p a b", b=128)), [bk, XT[s]], [XT[s]])

            load_w(0)
            load_x(0)
            transposes(0)
            cnt = [0]
            for e in range(NE):
                s = e % 2
                if e + 1 < NE:
                    load_w(e + 1)
                    load_x(e + 1)
                ti = 0
                for k in range(8):
                    for (n0, nsz) in nch:
                        u = ti % 2
                        ti += 1
                        bg = nf()
                        for c in range(8):
                            mm(bg[:, 0:nsz], W1b[s][:, c, k * 128:(k + 1) * 128], XT[s][:, c, n0:n0 + nsz], c == 0, c == 7,
                               [W1b[s], XT[s]], [bg])
                        bl = nf()
                        for c in range(8):
                            mm(bl[:, 0:nsz], W1b[s][:, c, D + k * 128:D + (k + 1) * 128], XT[s][:, c, n0:n0 + nsz], c == 0,
                               c == 7, [W1b[s], XT[s]], [bl])
                        ts("dve", tg_[u][:, 0:nsz], bg[:, 0:nsz], b1T[:, e * 16 + k:e * 16 + k + 1], 7.0, ALU.add, ALU.min,
                           [bg, b1T], [tg_[u]])
                        act(tsg[u][:, 0:nsz], tg_[u][:, 0:nsz], AF.Sigmoid, [tg_[u]], [tsg[u]], scale=1.702)
                        ts("dve", tl_[u][:, 0:nsz], bl[:, 0:nsz], b1P[:, e * 16 + 8 + k:e * 16 + 8 + k + 1], 8.0, ALU.add,
                           ALU.min, [bl, b1P], [tl_[u]])
                        tt("dve", tg_[u][:, 0:nsz], tg_[u][:, 0:nsz], tsg[u][:, 0:nsz], ALU.mult, [tg_[u], tsg[u]], [tg_[u]])
                        stt(AT[s][:, k, n0:n0 + nsz], tl_[u][:, 0:nsz], -6.0, tg_[u][:, 0:nsz], ALU.max, ALU.mult,
                            [tg_[u], tl_[u], AT[s]], [AT[s]])
                if e + 1 < NE:
                    transposes(e + 1)
                for blk in range(NBLK):
                    yi = cnt[0] % 2
                    cnt[0] += 1
                    r0 = e * CAP + blk * 128
                    for half in range(2):
                        bk = nf()
                        for k in range(8):
                            mm(bk[:, :], AT[s][:, k, blk * 128:(blk + 1) * 128], W2b[s][:, k, half * 512:(half + 1) * 512],
                               k == 0, False, [AT[s], W2b[s]], [bk])
                        mm(bk[:, :], onesb[0:1, :], b2b[s][0:1, half * 512:(half + 1) * 512], False, True,
                           [onesb, b2b[s]], [bk])
                        act(Ysb[yi][:, half * 512:(half + 1) * 512], bk[:, :], AF.Copy, [bk, Ysb[yi]], [Ysb[yi]])
                    dma("sp", ys_d[r0:r0 + 128, :], Ysb[yi][:, :], [Ysb[yi]], [], "yo%d" % yi, accw=[ys_db])
            S.barrier()
        stop_if(4)

        with ExitStack() as pc:
            ND = 3
            h1t = [sb(pc, "h1t%d" % i, [128, D], F32) for i in range(ND)]
            yj = [[sb(pc, "yj%d_%d" % (i, j), [128, D], F32) for j in range(4)] for i in range(ND)]
            acc = [sb(pc, "acc%d" % i, [128, D], F32) for i in range(ND)]
            ot = [sb(pc, "ot%d" % i, [128, D], F32) for i in range(ND)]
            lnt2 = [(sb(pc, "c_st6_0", [128, 2, 6], F32), sb(pc, "c_mv_0", [128, 2], F32),
                     sb(pc, "c_sc2_0", [128, 2], F32), sb(pc, "c_xn_0", [128, D], F32))] * ND
            for i in range(ND):
                for j in range(4):
                    E("dve", lambda e, i=i, j=j: e.memset(yj[i][j][:, :], 0.0), [], [yj[i][j]])

            def issue(t):
                i = t % ND
                dma("sp", h1t[i][:, :], h1_d[t * 128:(t + 1) * 128, :], [h1_db], [h1t[i]], "h1i%d" % i)
                for j in range(4):
                    E("pool", lambda e, i=i, j=j, t=t: e.indirect_dma_start(
                        out=yj[i][j][:, :], out_offset=None, in_=ys_d[:, :],
                        in_offset=bass.IndirectOffsetOnAxis(ap=idx_tab[:, t, j:j + 1], axis=0),
                        bounds_check=bc_reg, oob_is_err=False),
                      [idx_b[t], ys_db], [yj[i][j]], dsem="g%d_%d" % (i, j))

            for t in range(min(ND - 1, NT)):
                issue(t)
            for t in range(NT):
                i = t % ND
                if t + ND - 1 < NT:
                    issue(t + ND - 1)
                act(acc[i][:, :], h1t[i][:, :], AF.Copy, [h1t[i]], [acc[i]], scale=ALPHA)
                for j in range(4):
                    stt(acc[i][:, :], yj[i][j][:, :], g_tab[:, t, j:j + 1], acc[i][:, :], ALU.mult, ALU.add,
                        [yj[i][j], g_b[t], acc[i]], [acc[i]])
                layer_norm(lnt2[i], acc[i][:, :], acc[i], ln2g, ln2b, ot[i][:, :], ot[i])
                dma("sp", out_d[t * 128:(t + 1) * 128, :], ot[i][:, :], [ot[i]], [], "oo%d" % i, accw=[out_db])
            S.barrier()
        if os.environ.get("KDEBUG"):
            print("counts", S.count, {k: v[1] for k, v in S.dsem.items()}, "ninst", S.ninst)


def make_consts(CAP):
    cf = np.zeros((128, CF_TOT), np.float64)
    j = np.arange(128)[:, None]
    i = np.arange(128)[None, :]
    cf[:, CF_IDENT:CF_IDENT + 128] = (j == i)
    cf[:, CF_TRI:CF_TRI + 128] = (j <= i) * (-1.0 / 16.0)
    cf[:, CF_SUF:CF_SUF + 128] = (j > i) * (-1.0 / 16.0)
    cf[:, CF_LM:CF_LM + 128] = (j <= i)
    cf[:, CF_LM + 128:CF_LM + 256] = (j > i) & ((j // 64) == (i // 64))
    lg = np.log1p(-np.exp2(-5.0 - np.arange(4, dtype=np.float32)).astype(np.float32)).astype(np.float32).astype(np.float64)
    for h in range(4):
        same = (j // 64) == (i // 64)
        dt = np.where(same, np.exp(lg[h] * np.abs(i - j)), np.where(j < i, np.exp(lg[h] * (i - j)), 0.0))
        cf[:, CF_DT + h * 128:CF_DT + (h + 1) * 128] = dt
        cf[:, CF_DQ + h] = np.exp(lg[h] * (np.arange(128) + 1))
        cf[:, CF_DK + h] = np.exp(lg[h] * (127 - np.arange(128)))
    for hp in range(2):
        cf[0:64, CF_DECR + hp] = np.exp(lg[2 * hp] * 128)
        cf[64:128, CF_DECR + hp] = np.exp(lg[2 * hp + 1] * 128)
    cf[:, CF_STRICT:CF_STRICT + 128] = (j < i)
    cf[:, CF_ONES:CF_ONES + 128] = 1.0
    invf = (1.0 / (np.float32(10000.0) ** np.linspace(0.0, 1.0, 32, dtype=np.float32))).astype(np.float32)
    cf[:, CF_INVF:CF_INVF + 32] = invf[None, :]
    cf[:, CF_EOFF:CF_EOFF + 32] = (np.arange(32) * CAP + 1)[None, :]
    cf[:, CF_N16:CF_N16 + 2] = -1.0 / 16.0
    return cf.astype(np.float32)


def _col_perm():
    sizes = (256, 256, 512, 512, 16, 256, 256, 512, 512)
    offs = np.cumsum((0,) + sizes)
    blk = lambda b: np.arange(offs[b], offs[b + 1])
    return np.concatenate([blk(1), blk(6), blk(0), blk(5), blk(2), blk(7), blk(3), blk(8), blk(4)])


def prepare_inputs(inputs, n_cores, NT, NPRE, CAP, seq):
    f = lambda a: np.ascontiguousarray(np.asarray(a, dtype=np.float32))
    x = f(inputs["x"])
    pos = np.asarray(inputs["positions"]).astype(np.int32)
    perm = _col_perm()
    w_in = np.ascontiguousarray(f(inputs["w_in"])[0][:, perm])
    rowp = np.concatenate([f(inputs["ln_in_g"]).reshape(-1), f(inputs["ln_in_b"]).reshape(-1),
                           f(inputs["ln1_g"]).reshape(-1), f(inputs["ln1_b"]).reshape(-1),
                           f(inputs["ln2_g"]).reshape(-1), f(inputs["ln2_b"]).reshape(-1),
                           f(inputs["gla_norm_g"]).reshape(-1), f(inputs["ret_norm_g"]).reshape(-1),
                           f(inputs["ret_norm_b"]).reshape(-1), f(inputs["router_b"]).reshape(-1)])[None, :]
    b1 = f(inputs["moe_b1"])[0]
    b1T = np.ascontiguousarray(b1.reshape(NE, 16, 128).transpose(2, 0, 1).reshape(128, NE * 16))
    shared = {
        "cf": make_consts(CAP), "w_in": w_in, "w_out": f(inputs["w_out"])[0], "gate_w": f(inputs["gla_gate_w"])[0],
        "gate_b": f(inputs["gla_gate_b"]), "rowp": np.ascontiguousarray(rowp), "router_w": f(inputs["router_w"])[0],
        "moe_w1": f(inputs["moe_w1"])[0], "moe_b1T": b1T, "moe_w2": f(inputs["moe_w2"])[0], "moe_b2": f(inputs["moe_b2"])[0],
    }
    halves = seq // (NT * 128)
    in_maps = []
    for c in range(n_cores):
        b, hf = c // halves, c % halves
        s0 = hf * NT * 128
        m = dict(shared)
        m["x_own"] = np.ascontiguousarray(x[b, s0:s0 + NT * 128])
        if NPRE > 0:
            p0 = s0 - NPRE * 128 if hf > 0 else 0
            m["x_pre"] = np.ascontiguousarray(x[b, p0:p0 + NPRE * 128])
            ppre = pos[b, p0:p0 + NPRE * 128]
        else:
            m["x_pre"] = np.zeros((128, D), np.float32)
            ppre = np.zeros((0,), np.int32)
        pall = np.concatenate([ppre, pos[b, s0:s0 + NT * 128]])
        m["pos"] = np.ascontiguousarray(pall.reshape(-1, 128).T)
        m["flag"] = np.full((128, 1), 1.0 if hf > 0 else 0.0, np.float32)
        in_maps.append(m)
    return in_maps


def run(inputs, n_cores, NT, NPRE, CAP, seq, bsz):
    nc = build_program(NT, NPRE, CAP)
    in_maps = prepare_inputs(inputs, n_cores, NT, NPRE, CAP, seq)
    res = run_bass_kernel_spmd(nc, in_maps, core_ids=list(range(n_cores)))
    halves = seq // (NT * 128)
    out = np.zeros((bsz, seq, D), np.float32)
    for c in range(n_cores):
        b, hf = c // halves, c % halves
        out[b, hf * NT * 128:(hf + 1) * NT * 128] = res.results[c]["out"]
    return out


def kernel(**inputs):
    return run(inputs, 8, 32, 32, 640, 8192, 4)
```
